# Optimizing a Trainium2 kernel written in Bass

```python
import math
import jax
import jax.numpy as jnp
from jax import lax
import numpy as np

D_MODEL = 1024
BATCH = 8
SEQ = 2048
DEPTH = 2

N_MIXERS = 2
EPS = 1e-6

GDN_QK_HEADS = 8
GDN_V_HEADS = 16
GDN_HEAD_DIM = 128
GDN_CONV = 4
GDN_CHUNK = 64
GDN_QK_W = GDN_QK_HEADS * GDN_HEAD_DIM
GDN_V_W = GDN_V_HEADS * GDN_HEAD_DIM
GDN_CONV_W = 2 * GDN_QK_W + GDN_V_W
GDN_IN = GDN_CONV_W + GDN_V_W + 2 * GDN_V_HEADS

NSA_HEADS = 16
NSA_GROUPS = 4
NSA_HPG = NSA_HEADS // NSA_GROUPS
NSA_HEAD_DIM = 64
NSA_CMP_LEN = 32
NSA_CMP_STRIDE = 16
NSA_SLC_LEN = 64
NSA_TOP_K = 8
NSA_WINDOW = 512
NSA_QBLOCK = 128
NSA_Q_W = NSA_HEADS * NSA_HEAD_DIM
NSA_KV_W = NSA_GROUPS * NSA_HEAD_DIM
NSA_IN = 2 * NSA_Q_W + 6 * NSA_KV_W + 3 * NSA_HEADS

REL_BUCKETS = 32
REL_MAX_DIST = 128
NEG_INF = -1e30

kernel_name = 'hybrid_gdn_nsa_adaln'


def rms_norm(x, g):
    xf = x.astype(jnp.float32)
    y = xf * lax.rsqrt(jnp.mean(xf * xf, axis=-1, keepdims=True) + EPS)
    return (y * g.astype(jnp.float32)).astype(x.dtype)


def l2norm(x):
    return x * lax.rsqrt(jnp.sum(x * x, axis=-1, keepdims=True) + EPS)


def rel_bucket(dist):
    n = jnp.maximum(dist, 0)
    max_exact = REL_BUCKETS // 2
    nf = jnp.maximum(n, 1).astype(jnp.float32)
    large = max_exact + (jnp.log(nf / max_exact) / math.log(REL_MAX_DIST / max_exact)
                         * (REL_BUCKETS - max_exact)).astype(jnp.int32)
    large = jnp.minimum(large, REL_BUCKETS - 1)
    return jnp.where(n < max_exact, n, large)


def head_bias(rel_bias, dist):
    b = rel_bias[rel_bucket(dist)]
    return jnp.transpose(b, (2, 0, 1)).reshape(NSA_GROUPS, NSA_HPG, dist.shape[0], dist.shape[1])


def causal_depthwise_conv(x, w):
    kw, ch = w.shape
    return lax.conv_general_dilated(x, w[:, None, :].astype(x.dtype), window_strides=(1,),
                                    padding=[(kw - 1, 0)], dimension_numbers=('NWC', 'WIO', 'NWC'),
                                    feature_group_count=ch)


def gated_deltanet(h, w_in, conv_w, a_log, dt_bias, norm_w, w_out):
    bsz, t, _ = h.shape
    nh, dh, cs = GDN_V_HEADS, GDN_HEAD_DIM, GDN_CHUNK
    nc = t // cs
    f32 = jnp.float32
    proj = h @ w_in
    qkv, z, b_lin, a_lin = jnp.split(proj, [GDN_CONV_W, GDN_CONV_W + GDN_V_W, GDN_CONV_W + GDN_V_W + nh], axis=-1)
    qkv = jax.nn.silu(causal_depthwise_conv(qkv, conv_w)).astype(f32)
    q, k, v = jnp.split(qkv, [GDN_QK_W, 2 * GDN_QK_W], axis=-1)
    rep = nh // GDN_QK_HEADS
    q = jnp.repeat(l2norm(q.reshape(bsz, t, GDN_QK_HEADS, dh)), rep, axis=2) * (dh ** -0.5)
    k = jnp.repeat(l2norm(k.reshape(bsz, t, GDN_QK_HEADS, dh)), rep, axis=2)
    v = v.reshape(bsz, t, nh, dh)
    beta = jax.nn.sigmoid(b_lin.astype(f32))
    g = -jnp.exp(a_log.astype(f32)) * jax.nn.softplus(a_lin.astype(f32) + dt_bias.astype(f32))
    to_chunks = lambda u: u.reshape(bsz, nc, cs, nh, -1).transpose(0, 3, 1, 2, 4)
    q, k, v = to_chunks(q), to_chunks(k), to_chunks(v)
    beta = to_chunks(beta)[..., 0]
    gc = jnp.cumsum(to_chunks(g)[..., 0], axis=-1)
    idx = jnp.arange(cs)
    lower = idx[:, None] >= idx[None, :]
    strict = idx[:, None] > idx[None, :]
    decay = jnp.exp(jnp.where(lower, gc[..., :, None] - gc[..., None, :], -jnp.inf))
    kb = k * beta[..., None]
    a_mat = jnp.where(strict, jnp.einsum('bhncd,bhnsd->bhncs', kb, k) * decay, 0.0)
    eye = jnp.eye(cs, dtype=f32)
    l_mat = a_mat + eye
    t_mat = lax.linalg.triangular_solve(l_mat, jnp.broadcast_to(eye, l_mat.shape), left_side=True,
                                        lower=True, unit_diagonal=True)
    u = jnp.einsum('bhncs,bhnsd->bhncd', t_mat, v * beta[..., None])
    w = jnp.einsum('bhncs,bhnsd->bhncd', t_mat, kb * jnp.exp(gc)[..., None])
    qk = jnp.where(lower, jnp.einsum('bhncd,bhnsd->bhncs', q, k) * decay, 0.0)
    g_last = gc[..., -1]
    k_dec = k * jnp.exp(g_last[..., None] - gc)[..., None]
    q_dec = q * jnp.exp(gc)[..., None]
    xs = tuple(jnp.moveaxis(a, 2, 0) for a in (q_dec, qk, u, w, k_dec, jnp.exp(g_last)))

    def step(state, inp):
        qd, qkc, uc, wc, kd, gl = inp
        v_new = uc - jnp.einsum('bhcd,bhde->bhce', wc, state)
        o = jnp.einsum('bhcd,bhde->bhce', qd, state) + jnp.einsum('bhcs,bhse->bhce', qkc, v_new)
        state = state * gl[..., None, None] + jnp.einsum('bhcd,bhce->bhde', kd, v_new)
        return state, o

    s0 = jnp.zeros((bsz, nh, dh, dh), f32)
    _, o = lax.scan(step, s0, xs)
    o = o.transpose(1, 0, 3, 2, 4).reshape(bsz, t, nh, dh)
    o = rms_norm(o, norm_w) * jax.nn.silu(z.reshape(bsz, t, nh, dh).astype(f32))
    return o.reshape(bsz, t, nh * dh).astype(h.dtype) @ w_out


def native_sparse_attention(h, w_in, cmp_pos, cmp_w1, cmp_w2, rel_bias, w_out):
    bsz, t, _ = h.shape
    nh, ng, hpg, dh = NSA_HEADS, NSA_GROUPS, NSA_HPG, NSA_HEAD_DIM
    f32 = jnp.float32
    proj = h @ w_in
    q = proj[..., :NSA_Q_W].reshape(bsz, t, ng, hpg, dh).transpose(0, 2, 3, 1, 4) * (dh ** -0.5)
    kv = proj[..., NSA_Q_W:NSA_Q_W + 6 * NSA_KV_W].reshape(bsz, t, 6, ng, dh)
    kc, vc, ks, vs, kw, vw = [kv[:, :, i] for i in range(6)]
    off = NSA_Q_W + 6 * NSA_KV_W
    gates = jax.nn.sigmoid(proj[..., off:off + 3 * nh].astype(f32)).reshape(bsz, t, ng, hpg, 3)
    gates = gates.transpose(0, 2, 3, 1, 4)
    z = proj[..., off + 3 * nh:]

    n_cmp = (t - NSA_CMP_LEN) // NSA_CMP_STRIDE + 1
    tok = jnp.arange(n_cmp)[:, None] * NSA_CMP_STRIDE + jnp.arange(NSA_CMP_LEN)[None, :]

    def compress(xk, i):
        blk = xk[:, tok] + cmp_pos[i][None, None, :, None, :]
        blk = blk.transpose(0, 1, 3, 2, 4).reshape(bsz, n_cmp, ng, NSA_CMP_LEN * dh)
        return (jax.nn.silu(blk @ cmp_w1[i]) @ cmp_w2[i]).transpose(0, 2, 1, 3)

    k_cmp = compress(kc, 0)
    v_cmp = compress(vc, 1)
    cmp_end = jnp.arange(n_cmp) * NSA_CMP_STRIDE + NSA_CMP_LEN - 1

    n_slc = t // NSA_SLC_LEN
    k_sel = min(NSA_TOP_K, n_slc)
    c_start = jnp.arange(n_cmp)[:, None] * NSA_CMP_STRIDE
    s_start = jnp.arange(n_slc)[None, :] * NSA_SLC_LEN
    overlap = (jnp.clip(jnp.minimum(c_start + NSA_CMP_LEN, s_start + NSA_SLC_LEN) - jnp.maximum(c_start, s_start), 0)
               .astype(f32) / NSA_CMP_LEN)
    ks_blk = ks.reshape(bsz, n_slc, NSA_SLC_LEN, ng, dh).transpose(0, 3, 1, 2, 4)
    vs_blk = vs.reshape(bsz, n_slc, NSA_SLC_LEN, ng, dh).transpose(0, 3, 1, 2, 4)
    gather = jax.vmap(jax.vmap(lambda kb, ix: kb[ix]))
    tbl = rel_bias.T.reshape(ng, hpg, REL_BUCKETS)
    gi = jnp.arange(ng)[None, :, None, None, None]
    hi = jnp.arange(hpg)[None, None, :, None, None]
    blk_id = jnp.arange(n_slc)

    pad = ((0, 0), (0, 0), (NSA_WINDOW, 0), (0, 0))
    kw_p = jnp.pad(kw.transpose(0, 2, 1, 3), pad)
    vw_p = jnp.pad(vw.transpose(0, 2, 1, 3), pad)

    def query_block(bi):
        q0 = bi * NSA_QBLOCK
        qb = lax.dynamic_slice_in_dim(q, q0, NSA_QBLOCK, axis=3)
        gb = lax.dynamic_slice_in_dim(gates, q0, NSA_QBLOCK, axis=3)
        tq = q0 + jnp.arange(NSA_QBLOCK)
        dist = tq[:, None] - cmp_end[None, :]
        valid = dist >= 0
        s = jnp.einsum('bghqd,bgkd->bghqk', qb, k_cmp).astype(f32) + head_bias(rel_bias, dist)
        s = jnp.where(valid, s, NEG_INF)
        p_cmp = jax.nn.softmax(s, axis=-1) * jnp.any(valid, axis=-1)[:, None].astype(f32)
        o_cmp = jnp.einsum('bghqk,bgkd->bghqd', p_cmp.astype(v_cmp.dtype), v_cmp)
        imp = jnp.einsum('bghqk,ks->bgqs', p_cmp, overlap)
        cur = tq // NSA_SLC_LEN
        forced = (blk_id[None, :] == 0) | (blk_id[None, :] == cur[:, None]) | (blk_id[None, :] == cur[:, None] - 1)
        causal_blk = blk_id[None, :] * NSA_SLC_LEN <= tq[:, None]
        imp = jnp.where(forced, jnp.inf, jnp.where(causal_blk, imp, -jnp.inf))
        _, sel = lax.top_k(imp, k_sel)
        n_s = k_sel * NSA_SLC_LEN
        k_g = gather(ks_blk, sel).reshape(bsz, ng, NSA_QBLOCK, n_s, dh)
        v_g = gather(vs_blk, sel).reshape(bsz, ng, NSA_QBLOCK, n_s, dh)
        pos = (sel[..., None] * NSA_SLC_LEN + jnp.arange(NSA_SLC_LEN)).reshape(bsz, ng, NSA_QBLOCK, n_s)
        dist_s = tq[None, None, :, None] - pos
        s = jnp.einsum('bghqd,bgqsd->bghqs', qb, k_g).astype(f32) + tbl[gi, hi, rel_bucket(dist_s)[:, :, None]]
        s = jnp.where((dist_s >= 0)[:, :, None], s, NEG_INF)
        o_slc = jnp.einsum('bghqs,bgqsd->bghqd', jax.nn.softmax(s, axis=-1).astype(v_g.dtype), v_g)
        kwb = lax.dynamic_slice_in_dim(kw_p, q0, NSA_WINDOW + NSA_QBLOCK, axis=2)
        vwb = lax.dynamic_slice_in_dim(vw_p, q0, NSA_WINDOW + NSA_QBLOCK, axis=2)
        tk = q0 - NSA_WINDOW + jnp.arange(NSA_WINDOW + NSA_QBLOCK)
        dist_w = tq[:, None] - tk[None, :]
        valid_w = (dist_w >= 0) & (dist_w < NSA_WINDOW) & (tk[None, :] >= 0)
        s = jnp.einsum('bghqd,bgkd->bghqk', qb, kwb).astype(f32) + head_bias(rel_bias, dist_w)
        s = jnp.where(valid_w, s, NEG_INF)
        o_win = jnp.einsum('bghqk,bgkd->bghqd', jax.nn.softmax(s, axis=-1).astype(vwb.dtype), vwb)
        return (gb[..., 0:1] * o_cmp.astype(f32) + gb[..., 1:2] * o_slc.astype(f32)
                + gb[..., 2:3] * o_win.astype(f32))

    outs = lax.map(query_block, jnp.arange(t // NSA_QBLOCK))
    o = outs.transpose(1, 0, 4, 2, 3, 5).reshape(bsz, t, nh * dh)
    o = o * jax.nn.silu(z.astype(f32))
    return o.astype(h.dtype) @ w_out


def setup_inputs(seed: int = 0) -> dict:
    key = jax.random.key(seed)
    ks = jax.random.split(key, 20)
    n_a = (DEPTH + N_MIXERS - 1) // N_MIXERS
    n_b = DEPTH // N_MIXERS
    nrm = lambda k, shape, scale: jax.random.normal(k, shape, jnp.float32) * scale
    dt = jnp.exp(jax.random.uniform(ks[7], (n_a, GDN_V_HEADS), jnp.float32, math.log(1e-3), math.log(1e-1)))
    return {
        'x': nrm(ks[0], (BATCH, SEQ, D_MODEL), 1.0),
        'c': nrm(ks[1], (BATCH, D_MODEL), 1.0),
        'ada_w': nrm(ks[2], (DEPTH, D_MODEL, 3 * D_MODEL), D_MODEL ** -0.5),
        'ada_b': nrm(ks[3], (DEPTH, 3 * D_MODEL), 0.01),
        'norm_g': 1.0 + nrm(ks[4], (DEPTH, D_MODEL), 0.05),
        'gdn_w_in': nrm(ks[5], (n_a, D_MODEL, GDN_IN), D_MODEL ** -0.5),
        'gdn_conv_w': nrm(ks[6], (n_a, GDN_CONV, GDN_CONV_W), GDN_CONV ** -0.5),
        'gdn_a_log': jnp.log(jax.random.uniform(ks[8], (n_a, GDN_V_HEADS), jnp.float32, 1.0, 16.0)),
        'gdn_dt_bias': dt + jnp.log(-jnp.expm1(-dt)),
        'gdn_norm_w': 1.0 + nrm(ks[9], (n_a, GDN_HEAD_DIM), 0.05),
        'gdn_w_out': nrm(ks[10], (n_a, GDN_V_W, D_MODEL), GDN_V_W ** -0.5),
        'nsa_w_in': nrm(ks[11], (n_b, D_MODEL, NSA_IN), D_MODEL ** -0.5),
        'nsa_cmp_pos': nrm(ks[12], (n_b, 2, NSA_CMP_LEN, NSA_HEAD_DIM), 0.1),
        'nsa_cmp_w1': nrm(ks[13], (n_b, 2, NSA_CMP_LEN * NSA_HEAD_DIM, NSA_HEAD_DIM), (NSA_CMP_LEN * NSA_HEAD_DIM) ** -0.5),
        'nsa_cmp_w2': nrm(ks[14], (n_b, 2, NSA_HEAD_DIM, NSA_HEAD_DIM), NSA_HEAD_DIM ** -0.5),
        'nsa_w_out': nrm(ks[15], (n_b, NSA_Q_W, D_MODEL), NSA_Q_W ** -0.5),
        'rel_bias': nrm(ks[16], (REL_BUCKETS, NSA_HEADS), 0.5),
        'final_g': 1.0 + nrm(ks[17], (D_MODEL,), 0.05),
    }


def reference(x, c, ada_w, ada_b, norm_g, gdn_w_in, gdn_conv_w, gdn_a_log, gdn_dt_bias, gdn_norm_w, gdn_w_out,
              nsa_w_in, nsa_cmp_pos, nsa_cmp_w1, nsa_cmp_w2, nsa_w_out, rel_bias, final_g):
    cond = jax.nn.silu(c)
    for i in range(DEPTH):
        mod = cond @ ada_w[i] + ada_b[i]
        shift, scale, gate = jnp.split(mod, 3, axis=-1)
        hdn = rms_norm(x, norm_g[i]) * (1.0 + scale[:, None, :]) + shift[:, None, :]
        j = i // N_MIXERS
        if i % N_MIXERS == 0:
            y = gated_deltanet(hdn, gdn_w_in[j], gdn_conv_w[j], gdn_a_log[j], gdn_dt_bias[j], gdn_norm_w[j], gdn_w_out[j])
        else:
            y = native_sparse_attention(hdn, nsa_w_in[j], nsa_cmp_pos[j], nsa_cmp_w1[j], nsa_cmp_w2[j], rel_bias, nsa_w_out[j])
        x = x + gate[:, None, :] * y
    return rms_norm(x, final_g)
```

```python
import math
from contextlib import ExitStack

import numpy as np
import concourse.bass as bass
import concourse.mybir as mybir
from concourse.ap import AP
from concourse.bass_utils import run_bass_kernel_spmd

F32 = mybir.dt.float32
F32R = mybir.dt.float32r
BF16 = mybir.dt.bfloat16
AF = mybir.ActivationFunctionType
ALU = mybir.AluOpType

D = 1024
T = 2048
NT = 16
EPS = 1e-6
GDN_IN = 6176
NSA_IN = 3632
BIG = 30000.0


class Buf:
    __slots__ = ("name", "w", "r", "excl")

    def __init__(self, name="", excl=False):
        self.name = name
        self.w = {}
        self.r = []
        self.excl = excl


class Op:
    __slots__ = ("eng", "fn", "deps", "signal", "is_dma", "dma_id", "seq", "semval", "cost", "prio", "t0", "t1",
                 "nsucc", "succs", "npend", "ready", "tab")

    def __init__(self, eng, fn, is_dma=False, cost=0.3):
        self.eng = eng
        self.fn = fn
        self.deps = {}
        self.signal = False
        self.is_dma = is_dma
        self.dma_id = None
        self.seq = 0
        self.semval = None
        self.cost = cost
        self.prio = 0.0
        self.t0 = 0.0
        self.t1 = 0.0
        self.succs = []
        self.npend = 0
        self.ready = 0.0
        self.tab = None


ENGS = ("pe", "act", "dve", "pool", "sp")
CENGS = ("pe", "act", "dve", "pool")
ND = 32
SYNC_LAT = 0.30
DMA_LAT = 2.0


class Prog:
    def __init__(self, nc, schedule=True):
        self.nc = nc
        self.segs = [[]]
        self.nops = 0
        self.schedule = schedule

    def _add_dep(self, op, dep):
        if dep is op:
            return
        order_only = (dep.eng == "pe" and op.eng == "pe" and not dep.is_dma and not op.is_dma)
        if dep.is_dma and op.is_dma and False:
            return
        prev = op.deps.get(dep)
        if prev is None or (prev is False and not order_only):
            op.deps[dep] = not order_only

    def op(self, eng, fn, reads=(), writes=(), is_dma=False, cost=0.3):
        o = Op(eng, fn, is_dma, cost)
        o.seq = self.nops
        self.nops += 1
        if any(b.excl for b in reads):
            writes = list(writes) + [b for b in reads if b.excl]
            reads = [b for b in reads if not b.excl]
        for b in reads:
            for d in b.w.values():
                self._add_dep(o, d)
        for b in writes:
            for d in b.w.values():
                self._add_dep(o, d)
            for d in b.r:
                self._add_dep(o, d)
        key = ("dma", o.seq) if is_dma else eng
        for b in reads:
            b.r.append(o)
        for b in writes:
            b.w = {key: o}
            b.r = []
        self.segs[-1].append(o)
        return o

    def barrier(self):
        if self.segs[-1]:
            self.segs.append([])

    def _schedule_segment(self, ops):
        import heapq
        inseg = set(ops)
        for o in ops:
            o.succs = []
        for o in ops:
            o.deps = {d: s for d, s in o.deps.items() if d in inseg}
            o.npend = len(o.deps)
            for d in o.deps:
                d.succs.append(o)
        if not self.schedule:
            return {e: [o for o in ops if o.eng == e] for e in ENGS}
        for o in reversed(ops):
            m = 0.0
            for s in o.succs:
                if s.prio > m:
                    m = s.prio
            o.prio = m + (DMA_LAT if o.is_dma else o.cost)
            o.ready = 0.0
        free = {e: 0.0 for e in ENGS}
        hA = {e: [] for e in ENGS}
        hB = {e: {} for e in ENGS}
        cur_tab = [None]
        TAB_PEN, TAB_COST = 6.0, 1.3
        for o in ops:
            if o.npend == 0:
                heapq.heappush(hA[o.eng], (0.0, -o.prio, o.seq, o))
        out = {e: [] for e in ENGS}
        remaining = len(ops)
        while remaining:
            best = None
            for e in ENGS:
                a, b = hA[e], hB[e]
                while a and a[0][0] <= free[e]:
                    _, np_, sq, o = heapq.heappop(a)
                    heapq.heappush(b.setdefault(o.tab, []), (np_, sq, o))
                bt = None
                for tb_, hp in b.items():
                    if not hp:
                        continue
                    sc = -hp[0][0] - (TAB_PEN if (e == "act" and tb_ is not None and tb_ != cur_tab[0]) else 0.0)
                    if bt is None or sc > bt[0]:
                        bt = (sc, tb_)
                if bt is not None:
                    cand = (free[e], -bt[0], e, bt[1], True)
                elif a:
                    cand = (a[0][0], a[0][1], e, None, False)
                else:
                    continue
                if best is None or cand[:2] < best[:2]:
                    best = cand
            st, _, e, tb_, fromb = best
            if fromb:
                _, _, o = heapq.heappop(hB[e][tb_])
            else:
                _, _, _, o = heapq.heappop(hA[e])
            if e == "act" and o.tab is not None and o.tab != cur_tab[0]:
                st += TAB_COST
                cur_tab[0] = o.tab
            o.t0 = st
            if o.is_dma:
                free[e] = st + 0.06
                o.t1 = st + DMA_LAT + o.cost
            else:
                o.t1 = st + o.cost
                free[e] = o.t1
            out[e].append(o)
            remaining -= 1
            for s_ in o.succs:
                t = (o.t1 + SYNC_LAT) if s_.deps[o] else o.t0 + 0.01
                if t > s_.ready:
                    s_.ready = t
                s_.npend -= 1
                if s_.npend == 0:
                    heapq.heappush(hA[s_.eng], (s_.ready, -s_.prio, s_.seq, s_))
        self.est_time = getattr(self, "est_time", 0.0) + max(o.t1 for o in ops)
        return out

    def emit(self, final_dma_ops=()):
        nc = self.nc
        segs = [sg for sg in self.segs if sg]
        order = {e: [] for e in ENGS}
        dma_order = []
        for sg in segs:
            sch = self._schedule_segment(sg)
            seg_dmas = [o for o in sg if o.is_dma]
            seg_dmas.sort(key=lambda o: (o.t0, o.seq))
            dma_order += seg_dmas
            lasts = {}
            for e in CENGS:
                for o in reversed(sch[e]):
                    if not o.is_dma:
                        lasts[e] = o
                        break
            for e in ENGS:
                order[e] += [("op", o) for o in sch[e]]
                order[e].append(("bar", (lasts, seg_dmas)))
            for o in lasts.values():
                o.signal = True
        for j, o in enumerate(dma_order):
            o.dma_id = j
        for e in ENGS:
            for kind, o in order[e]:
                if kind == "op":
                    for d, needs in o.deps.items():
                        if needs and not d.is_dma:
                            d.signal = True
        with ExitStack() as es:
            sems = {e: es.enter_context(nc.semaphore("s_" + e)) for e in CENGS}
            dsems = [es.enter_context(nc.semaphore("d%d" % i)) for i in range(ND)]
            for e in ENGS:
                c = 0
                for kind, o in order[e]:
                    if kind != "op" or o.is_dma:
                        continue
                    if o.signal:
                        c += 1
                    o.semval = c
            block = es.enter_context(nc.Block())
            deco = {"pe": block.tensor, "act": block.scalar, "dve": block.vector,
                    "pool": block.gpsimd, "sp": block.sync}

            def run_engine(ename):
                def body(eng):
                    waited = {}

                    def wait(sem, val):
                        if waited.get(sem.name, -1) >= val:
                            return
                        waited[sem.name] = val
                        eng.wait_ge(sem, val)

                    def wait_dma(d):
                        j = d.dma_id
                        wait(dsems[j % ND], 16 * (j // ND + 1))

                    for kind, o in order[ename]:
                        if kind == "bar":
                            lasts, seg_dmas = o
                            for e2, lo in lasts.items():
                                wait(sems[e2], lo.semval)
                            for d in seg_dmas:
                                wait_dma(d)
                            continue
                        for d, needs in o.deps.items():
                            if not needs:
                                continue
                            if d.is_dma:
                                wait_dma(d)
                            else:
                                wait(sems[d.eng], d.semval)
                        if o.is_dma:
                            j = o.dma_id
                            if j >= ND:
                                wait(dsems[j % ND], 16 * (j // ND))
                            o.fn(eng).then_inc(dsems[j % ND], 16)
                        else:
                            ins = o.fn(eng)
                            if o.signal:
                                ins.then_inc(sems[ename], 1)
                    if ename == "sp":
                        for o in final_dma_ops:
                            wait_dma(o)
                return body

            for ename in ENGS:
                deco[ename](run_engine(ename))

    @staticmethod
    def _n(ap):
        try:
            return ap.free_size()
        except Exception:
            return 128

    def mm(self, out, lhsT, rhs, start=True, stop=True, reads=(), writes=(), **kw):
        n = self._n(out)
        f32 = (lhsT.dtype == F32)
        cost = max(n, 48) * (4.0 if f32 else 1.0) / 1900.0 + 0.02
        return self.op("pe", lambda e: e.matmul(out, lhsT=lhsT, rhs=rhs, start=start, stop=stop, **kw), reads, writes, cost=cost)

    def tr(self, out, in_, ident, reads=(), writes=()):
        cost = 128 * (2.0 if in_.dtype == F32 else 1.0) / 1900.0 + 0.05
        return self.op("pe", lambda e: e.transpose(out=out, in_=in_, identity=ident), reads, writes, cost=cost)

    _TABS = {AF.Exp: "exp", AF.Silu: "silu", AF.Sqrt: "sqrt", AF.Sigmoid: "sigmoid", AF.Ln: "ln", AF.Tanh: "exp"}

    def actv(self, out, in_, func, reads=(), writes=(), **kw):
        o = self.op("act", lambda e: e.activation(out=out, in_=in_, func=func, **kw), reads, writes, cost=0.28 + self._n(out) * 0.00085)
        o.tab = self._TABS.get(func)
        return o

    def _vcost(self, eng, out):
        n = self._n(out)
        if eng == "pool":
            return 0.5 + n * 0.0021
        if eng == "act":
            return 0.28 + n * 0.00085
        return 0.2 + n * 0.00105

    def copy(self, eng, out, in_, reads=(), writes=()):
        if eng == "act":
            return self.op("act", lambda e: e.activation(out=out, in_=in_, func=AF.Copy), reads, writes, cost=self._vcost(eng, out))
        return self.op(eng, lambda e: e.tensor_copy(out=out, in_=in_), reads, writes, cost=self._vcost(eng, out))

    def tt(self, eng, out, in0, in1, op, reads=(), writes=()):
        return self.op(eng, lambda e: e.tensor_tensor(out=out, in0=in0, in1=in1, op=op), reads, writes, cost=self._vcost(eng, out))

    def ts(self, eng, out, in0, s1, s2, op0, op1=None, reads=(), writes=()):
        if eng == "act":
            assert op0 == ALU.mult and op1 is None
            return self.op("act", lambda e: e.activation(out=out, in_=in0, func=AF.Copy, scale=s1), reads, writes, cost=self._vcost(eng, out))
        if op1 is None:
            return self.op(eng, lambda e: e.tensor_scalar(out=out, in0=in0, scalar1=s1, scalar2=None, op0=op0), reads, writes,
                           cost=self._vcost(eng, out))
        return self.op(eng, lambda e: e.tensor_scalar(out=out, in0=in0, scalar1=s1, scalar2=s2, op0=op0, op1=op1), reads, writes,
                       cost=self._vcost(eng, out))

    def stt(self, eng, out, in0, scalar, in1, op0, op1, reads=(), writes=()):
        return self.op(eng, lambda e: e.scalar_tensor_tensor(out=out, in0=in0, scalar=scalar, in1=in1, op0=op0, op1=op1), reads, writes,
                       cost=self._vcost(eng, out))

    def dma(self, out, in_, reads=(), writes=(), q="sp", **kw):
        try:
            nbytes = out.size() * mybir.dt.size(out.dtype)
        except Exception:
            nbytes = 1 << 16
        return self.op(q, lambda e: e.dma_start(out=out, in_=in_, **kw), reads, writes, is_dma=True, cost=nbytes / 100e3)


class Arena:
    def __init__(self, nc):
        self.nc = nc
        self.base = (nc.sbuf_base + 31) // 32 * 32
        self.top = nc.sbuf_top
        self.off = self.base
        self.n = 0
        self.prog = None

    def alloc(self, shape, dtype, name=None):
        sz = int(np.prod(shape[1:])) * mybir.dt.size(dtype)
        sz = (sz + 31) // 32 * 32
        if self.off + sz > self.top:
            raise RuntimeError("SBUF arena overflow at %s: need %d, have %d" % (name, sz, self.top - self.off))
        self.n += 1
        self.peak = max(getattr(self, "peak", 0), self.off + sz - self.base + (self.nc.sbuf_top - self.top))
        t = self.nc.alloc_sbuf_tensor_at("%s_%d" % (name or "t", self.n), list(shape), dtype, offset=self.off)
        self.off += sz
        return t

    def alloc_top(self, shape, dtype, name=None):
        sz = int(np.prod(shape[1:])) * mybir.dt.size(dtype)
        sz = (sz + 31) // 32 * 32
        if self.top - sz < self.off:
            raise RuntimeError("SBUF arena overflow (top) at %s: need %d, have %d" % (name, sz, self.top - self.off))
        self.n += 1
        self.top -= sz
        return self.nc.alloc_sbuf_tensor_at("%s_%d" % (name or "t", self.n), list(shape), dtype, offset=self.top)

    def mark(self):
        return self.off

    def release(self, m):
        self.off = m
        if self.prog is not None:
            self.prog.barrier()


def bc(ap, shape):
    return ap.broadcast_to(list(shape))


CST_NAMES = ["ident", "ones", "u64", "onesbd", "ones_a", "ones_b", "mask_a", "mask_q"]


def make_consts():
    p = np.arange(128)
    same = (p[:, None] // 64) == (p[None, :] // 64)
    c = {}
    c["ident"] = np.eye(128)
    c["ones"] = np.ones((128, 128))
    c["u64"] = ((p[:, None] <= p[None, :]) & same) * 1.0
    c["onesbd"] = same * 1.0
    c["ones_a"] = np.repeat((p < 64)[:, None] * 1.0, 128, 1)
    c["ones_b"] = np.repeat((p >= 64)[:, None] * 1.0, 128, 1)
    c["mask_a"] = np.where((p[None, :] < p[:, None]) & same, 0.0, BIG)
    c["mask_q"] = np.where((p[None, :] >= p[:, None]) & same, 0.0, -BIG)
    return np.stack([c[n] for n in CST_NAMES], axis=1).astype(np.float32)


class _Stop(Exception):
    pass


def build(mode="all", dbg=None, neumann_dt=F32, stop=None, taps=()):
    nc = bass.Bass("TRN2", target_bir_lowering=False)
    tap_ops = []

    def tap(name, ap, reads):
        if name not in taps:
            return
        dt_ = nc.dram_tensor(name, list(ap.shape), ap.dtype, kind="ExternalOutput").ap()
        tap_ops.append(P.dma(dt_, ap, reads=reads, q="sp"))

    def ck(name):
        if stop == name:
            raise _Stop()

    dram = {}

    def din(name, shape, dt=F32):
        dram[name] = nc.dram_tensor(name, list(shape), dt, kind="ExternalInput").ap()
        return dram[name]

    x_in = din("x", [T, D])
    cvec = din("cvec", [128, 8])
    ada_w = din("ada_w", [2, D, 3 * D])
    ada_b = din("ada_b", [2, 128, 24])
    ada_b_row = din("ada_b_row", [2, 3 * D])
    norm_g = din("norm_g", [2, 128, 8])
    gdn_w_in = din("gdn_w_in", [D, GDN_IN])
    gdn_cw = din("gdn_cw", [128, 32, 4])
    gdn_alog = din("gdn_alog", [1, 16])
    gdn_dtb = din("gdn_dtb", [1, 16])
    gdn_normw = din("gdn_normw", [1, 128])
    gdn_w_out = din("gdn_w_out", [2048, D])
    final_g = din("final_g", [1, D])
    nsa_w = din("nsa_w", [D, 4144])
    nsa_w1 = din("nsa_w1", [2, 2048, 64])
    nsa_w2 = din("nsa_w2", [2, 64, 64])
    nsa_pos = din("nsa_pos", [2, 32, 64])
    nsa_wo = din("nsa_wo", [D, D])
    rel_bias_d = din("rel_bias", [32, 16])
    nsa_oh = din("nsa_oh", [33, 384])
    nsa_sh = din("nsa_sh", [128, NT, 127])
    nsa_em = din("nsa_em", [128, NT, 128])
    nsa_wm = din("nsa_wm", [128, 128])
    nsa_cmask = din("nsa_cmask", [128, NT, 32])
    nsa_fbias = din("nsa_fbias", [128, NT, 32])
    nsa_ovl = din("nsa_ovl", [127, 32])
    x1_d = nc.dram_tensor("x1_scratch", [T, D], F32).ap()
    og_d = nc.dram_tensor("og_scratch", [T, 2048], BF16).ap()
    zt_d = nc.dram_tensor("zt_scratch", [16, 128, 400], F32).ap()
    cst_d = din("cst", [128, len(CST_NAMES), 128])
    out_d = nc.dram_tensor("out", [T, D], F32, kind="ExternalOutput").ap()
    dbg_d = None
    if dbg is not None:
        dbg_d = nc.dram_tensor("dbg", list(dbg), F32, kind="ExternalOutput").ap()

    import os
    P = Prog(nc, schedule=(os.environ.get("KSCHED", "1") == "1"))
    A = Arena(nc)
    A.prog = P
    es = ExitStack()
    psum = [es.enter_context(nc.psum_tensor("ps%d" % i, [128, 512], F32)) for i in range(8)]
    final_ops = []

    cst = A.alloc([128, len(CST_NAMES), 128], F32, "cst")
    Bcst = Buf("cst")
    P.dma(cst[:], cst_d, writes=[Bcst])
    C = {n: cst[:, i, :] for i, n in enumerate(CST_NAMES)}
    identb = A.alloc([128, 128], BF16, "identb")
    onesb = A.alloc([128, 128], BF16, "onesb")
    P.copy("dve", identb[:], C["ident"], reads=[Bcst], writes=[Bcst])
    P.copy("dve", onesb[:], C["ones"], reads=[Bcst], writes=[Bcst])

    cond = A.alloc([128, 8], F32, "cond")
    modsb = A.alloc([128, 2, 24], F32, "mod")
    sc1 = A.alloc([128, 2, 8], F32, "sc1")
    adab = A.alloc([128, 2, 24], F32, "adab")
    normg = A.alloc([128, 2, 8], F32, "normg")
    gate_bc = A.alloc([128, D], F32, "gate_bc")
    Bsm = Buf("small")
    Bmod = Buf("mod")
    P.dma(cond[:], cvec, writes=[Bsm])
    P.dma(adab[:], ada_b.rearrange("l p j -> p l j"), writes=[Bsm])
    P.dma(normg[:], norm_g.rearrange("l p j -> p l j"), writes=[Bsm])
    P.actv(cond[:], cond[:], AF.Silu, reads=[Bsm], writes=[Bsm])
    Bps = [Buf("ps%d" % i, excl=True) for i in range(8)]
    Bmrow = Buf("modrow")

    def stage_a(wsa, Bwsa, modrow, adabrow, layers, nblk0=6):
        P.dma(adabrow[:], ada_b_row.rearrange("(o l) n -> o l n", o=1), writes=[Bmrow])
        cnt = 0
        for l in layers:
            nb_ = nblk0 if l == 0 else 6
            for cb_ in range(nb_):
                i = cnt % 2
                cnt += 1
                for kh in range(2):
                    P.dma(wsa[i][:, kh * 4:(kh + 1) * 4, :],
                          ada_w[l, kh * 512:(kh + 1) * 512, cb_ * 512:(cb_ + 1) * 512].rearrange("(kc p) n -> p kc n", p=128),
                          writes=[Bwsa[i]], q=("sp" if kh == 0 else "pool"))
                pb = i
                for kc in range(8):
                    P.mm(psum[pb][0:1, :], cond[:, kc:kc + 1], wsa[i][:, kc, :], start=(kc == 0), stop=(kc == 7),
                         reads=[Bwsa[i], Bsm], writes=[Bps[pb]])
                P.tt("dve", modrow[:, cb_ * 512:(cb_ + 1) * 512], psum[pb][0:1, :], adabrow[:, l, cb_ * 512:(cb_ + 1) * 512], ALU.add,
                     reads=[Bps[pb], Bmrow], writes=[Bmrow])
            nj = nb_ * 4
            for j in range(nj):
                P.mm(psum[2][:, j:j + 1], modrow[0:1, j * 128:(j + 1) * 128], C["ones"][0:1, 0:1], reads=[Bmrow, Bcst], writes=[Bps[2]])
            P.copy("dve", modsb[:, l, 0:nj], psum[2][:, 0:nj], reads=[Bps[2]], writes=[Bmod])
            P.stt("dve", sc1[:, l, :], modsb[:, l, 8:16], 1.0, normg[:, l, :], ALU.add, ALU.mult, reads=[Bmod, Bsm], writes=[Bmod])

    def stage_a_col(l, wsc, Bwsc, fcs=range(24), with_sc1=True):
        for fc in fcs:
            i = fc % 2
            for kh in range(2):
                P.dma(wsc[i][:, kh * 4:(kh + 1) * 4, :],
                      ada_w[l, kh * 512:(kh + 1) * 512, fc * 128:(fc + 1) * 128].rearrange("(kc p) n -> p kc n", p=128),
                      writes=[Bwsc[i]], q="sp")
            for kc in range(8):
                P.mm(psum[0][:, 0:1], wsc[i][:, kc, :], cond[:, kc:kc + 1], start=(kc == 0), stop=(kc == 7),
                     reads=[Bwsc[i], Bsm], writes=[Bps[0]])
            P.tt("dve", modsb[:, l, fc:fc + 1], psum[0][:, 0:1], adab[:, l, fc:fc + 1], ALU.add, reads=[Bps[0], Bsm], writes=[Bmod])
        if with_sc1:
            P.stt("dve", sc1[:, l, :], modsb[:, l, 8:16], 1.0, normg[:, l, :], ALU.add, ALU.mult, reads=[Bmod, Bsm], writes=[Bmod])
            stage_a_done[l] = True

    stage_a_done = [False, False]
    stage_a_mark = [None]

    def run_stage_a(layers):
        layers = [l_ for l_ in layers if not stage_a_done[l_]]
        stage_a_mark[0] = None
        if not layers:
            return
        for l_ in layers:
            stage_a_done[l_] = True
        stage_a_mark[0] = A.mark()
        wsa = [A.alloc([128, 8, 512], F32, "wsa%d" % i) for i in range(2)]
        modrow = A.alloc([1, 3 * D], F32, "modrow")
        adabrow = A.alloc([1, 2, 3 * D], F32, "adabrow")
        stage_a(wsa, [Buf(), Buf()], modrow, adabrow, layers, nblk0=(4 if mode == "all" else 6))

    def make_gate_bc(l):
        m = A.mark()
        tmp = A.alloc([128, 8, 128], F32, "gtmp")
        Bt = Buf()
        P.tt("pool", tmp[:], bc(C["ident"].unsqueeze(1), [128, 8, 128]),
             bc(modsb[:, l, 16:24].unsqueeze(2), [128, 8, 128]), ALU.mult, reads=[Bcst, Bmod], writes=[Bt])
        for hh in range(2):
            P.mm(psum[hh][:, :], C["ones"], tmp[:, hh * 4:(hh + 1) * 4, :].rearrange("p a b -> p (a b)"),
                 reads=[Bt, Bcst], writes=[Bps[hh]])
            P.copy("act", gate_bc[:, hh * 512:(hh + 1) * 512], psum[hh][:, :], reads=[Bps[hh]], writes=[Bmod])
        A.release(m)

    def make_hT(src_d, l, hT, BhT):
        m = A.mark()
        NX = 3
        xt = [A.alloc([128, D], F32, "xt%d" % i) for i in range(NX)]
        Bxt = [Buf() for _ in range(NX)]
        xs = [A.alloc([128, D], BF16, "xs%d" % i) for i in range(NX)]
        Bxs = [Buf() for _ in range(NX)]
        junk = A.alloc([128, D], BF16, "junk")
        Bj = Buf()
        st = A.alloc([128, NT, 3], F32, "st")
        Bst = [Buf() for _ in range(NT)]
        tmp = [A.alloc([128, 8, 128], F32, "htmp%d" % i) for i in range(NX)]
        Btmp = [Buf() for _ in range(NX)]
        for t in range(NT):
            i = t % NX
            P.dma(xt[i][:], src_d[t * 128:(t + 1) * 128, :], writes=[Bxt[i]], q=("sp" if t % 2 == 0 else "pool"))
            P.actv(junk[:], xt[i][:], AF.Square, reads=[Bxt[i]], writes=[Bj, Bst[t]], accum_out=st[:, t, 0:1])
            P.actv(st[:, t, 1:2], st[:, t, 0:1], AF.Sqrt, reads=[Bst[t]], writes=[Bst[t]], scale=1.0 / D, bias=EPS)
            P.op("dve", lambda e, t=t: e.reciprocal(out=st[:, t, 2:3], in_=st[:, t, 1:2]), reads=[Bst[t]], writes=[Bst[t]])
            P.actv(xs[i][:], xt[i][:], AF.Copy, reads=[Bxt[i], Bst[t]], writes=[Bxs[i]], scale=st[:, t, 2:3])
            pb = 2 + t % 3
            pst = psum[pb][:].bitcast(BF16)
            for fc in range(8):
                P.tr(pst[:, fc * 128:(fc + 1) * 128], xs[i][:, fc * 128:(fc + 1) * 128], identb[:],
                     reads=[Bxs[i], Bcst], writes=[Bps[pb]])
            P.tt("dve", tmp[i][:], pst[:, 0:1024].rearrange("p (a b) -> p a b", a=8),
                 bc(sc1[:, l, :].unsqueeze(2), [128, 8, 128]), ALU.mult, reads=[Bps[pb], Bmod], writes=[Btmp[i]])
            P.tt("dve" if t % 3 else "pool", hT[:, :, t * 128:(t + 1) * 128], tmp[i][:],
                 bc(modsb[:, l, 0:8].unsqueeze(2), [128, 8, 128]), ALU.add, reads=[Btmp[i], Bmod], writes=[BhT])
        A.release(m)

    def residual_out(src_d, dst_d, y_fn, final=False):
        pass

    def layer_gdn(src_d, dst_d):
        l = 0
        mL = A.mark()
        hT = A.alloc([128, 8, T], BF16, "hT")
        BhT = Buf("hT")
        run_stage_a([0] if mode == "all" else [0, 1])
        make_hT(src_d, l, hT, BhT)
        A.off = stage_a_mark[0] if stage_a_mark[0] is not None else A.off
        tap("hT", hT[:], [BhT])
        ck("B")
        if mode != "all":
            make_gate_bc(l)
        tap("gate_bc", gate_bc[:], [Bmod])
        ck("B2")
        wo_alias = hT[:].rearrange("p a (b c) -> p (a b) c", c=D)

        NS = 8
        sca = A.alloc([128, NS, NT, 16], F32, "sca")
        Bsc = Buf("sca")
        BETA, G, GC, GLT, GLA, GLB, BK, EKD = range(8)
        vecs = A.alloc([128, 3, 16], F32, "vecs")
        normw_bc = A.alloc([128, 128], F32, "normw_bc")
        cw = A.alloc([128, 32, 4], F32, "cw")
        Bv = Buf("vecs")
        P.dma(vecs[:, 0, :], AP(gdn_dtb.tensor, 0, [[0, 128], [1, 16]]), writes=[Bv])
        P.dma(vecs[:, 1, :], AP(gdn_alog.tensor, 0, [[0, 128], [1, 16]]), writes=[Bv])
        P.dma(normw_bc[:], AP(gdn_normw.tensor, 0, [[0, 128], [1, 128]]), writes=[Bv])
        P.dma(cw[:], gdn_cw, writes=[Bv])
        P.actv(vecs[:, 1, :], vecs[:, 1, :], AF.Exp, reads=[Bv], writes=[Bv])
        P.ts("dve", vecs[:, 1, :], vecs[:, 1, :], -1.0, None, ALU.mult, reads=[Bv], writes=[Bv])
        m1 = A.mark()
        wba = A.alloc([128, 8, 32], BF16, "wba")
        Bw = Buf()
        P.dma(wba[:], gdn_w_in[:, 6144:6176].rearrange("(kc p) n -> p kc n", p=128), writes=[Bw], q="pool")
        lin = A.alloc([128, NT, 32], F32, "lin")
        Bl = Buf()
        for t in range(NT):
            pb = t % 2
            for kc in range(8):
                P.mm(psum[pb][:, 0:32], hT[:, kc, t * 128:(t + 1) * 128], wba[:, kc, :], start=(kc == 0), stop=(kc == 7),
                     reads=[BhT, Bw], writes=[Bps[pb]])
            P.copy("dve", lin[:, t, :], psum[pb][:, 0:32], reads=[Bps[pb]], writes=[Bl])
        P.actv(sca[:, BETA], lin[:, :, 0:16], AF.Sigmoid, reads=[Bl], writes=[Bsc])
        P.tt("dve", sca[:, G], lin[:, :, 16:32], bc(vecs[:, 0, :].unsqueeze(1), [128, NT, 16]), ALU.add, reads=[Bl, Bv], writes=[Bsc])
        P.actv(sca[:, G], sca[:, G], AF.Exp, reads=[Bsc], writes=[Bsc])
        P.actv(sca[:, G], sca[:, G], AF.Ln, reads=[Bsc], writes=[Bsc], bias=1.0)
        P.tt("dve", sca[:, G], sca[:, G], bc(vecs[:, 1, :].unsqueeze(1), [128, NT, 16]), ALU.mult, reads=[Bsc, Bv], writes=[Bsc])
        gflat = sca[:, G].rearrange("p t h -> p (t h)")
        for i, (nm, dst) in enumerate((("u64", GC), ("onesbd", GLT), ("ones_a", GLA), ("ones_b", GLB))):
            pb = i % 2
            P.mm(psum[pb][:, 0:256], C[nm], gflat, reads=[Bsc, Bcst], writes=[Bps[pb]])
            P.copy("dve", sca[:, dst].rearrange("p t h -> p (t h)"), psum[pb][:, 0:256], reads=[Bps[pb]], writes=[Bsc])
        P.tt("dve", sca[:, EKD], sca[:, GLT], sca[:, GC], ALU.subtract, reads=[Bsc], writes=[Bsc])
        P.actv(sca[:, EKD], sca[:, EKD], AF.Exp, reads=[Bsc], writes=[Bsc])
        P.actv(sca[:, BK], sca[:, GC], AF.Exp, reads=[Bsc], writes=[Bsc])
        P.tt("dve", sca[:, BK], sca[:, BK], sca[:, BETA], ALU.mult, reads=[Bsc], writes=[Bsc])
        P.actv(sca[:, GLA], sca[:, GLA], AF.Exp, reads=[Bsc], writes=[Bsc])
        P.actv(sca[:, GLB], sca[:, GLB], AF.Exp, reads=[Bsc], writes=[Bsc])
        P.ts("dve", sca[:, GLT], sca[:, BETA], -1.0, None, ALU.mult, reads=[Bsc], writes=[Bsc])
        NBETA = GLT
        tap("sca", sca[:], [Bsc])
        A.release(m1)
        ck("C0")

        mG = A.mark()
        wbf_ = [A.alloc([128, 8, 768], BF16, "wbf%d" % i) for i in range(2)]
        Bwbf_ = [Buf("wbf0"), Buf("wbf1")]

        def load_gdn_w(g_):
            cols_ = [(g_ * 128, 128, 0), (1024 + g_ * 128, 128, 128), (2048 + g_ * 256, 256, 256), (4096 + g_ * 256, 256, 512)]
            for (c0, n, o0) in cols_:
                P.dma(wbf_[g_ % 2][:, :, o0:o0 + n], gdn_w_in[:, c0:c0 + n].rearrange("(kc p) n -> p kc n", p=128),
                      writes=[Bwbf_[g_ % 2]], q="pool")

        load_gdn_w(0)
        if mode == "all":
            wsc = [A.alloc([128, 8, 128], F32, "wsc%d" % i) for i in range(2)]
            Bwsc = [Buf(), Buf()]
            stage_a_col(0, wsc, Bwsc, fcs=range(16, 24), with_sc1=False)
            stage_a_col(1, wsc, Bwsc)
        dg = A.alloc([128, 4, 4, 128], BF16, "dg")
        Bdg = Buf("dg")
        qkT_g = [A.alloc([128, 2, T], BF16, "qkT%d" % i) for i in range(2)]
        BqkT_g = [Buf(), Buf()]
        ktok_g = [A.alloc([128, NT, 128], BF16, "ktok%d" % i) for i in range(2)]
        Bktok_g = [Buf(), Buf()]
        vtok_g = [A.alloc([128, NT, 2, 128], BF16, "vtok%d" % i) for i in range(2)]
        Bvtok_g = [Buf(), Buf()]
        zs_g = [A.alloc([128, NT, 2, 128], BF16, "zs%d" % i) for i in range(2)]
        Bzs_g = [Buf(), Buf()]
        ogun_g = [A.alloc([128, NT, 2, 128], BF16, "ogun%d" % i) for i in range(2)]
        Bog_g = [Buf(), Buf()]
        ssq_g = [A.alloc([128, NT, 2], F32, "ssq%d" % i) for i in range(2)]
        Bssq_g = [Buf(), Buf()]
        pre_ = [A.alloc([128, 3 + T], BF16, "pre%d" % i) for i in range(2)]
        Bpre_ = [Buf("pre0"), Buf("pre1")]
        NS1 = 2
        s1 = [A.alloc([128, 512], F32, "s1_%d" % i) for i in range(NS1)]
        Bs1 = [Buf() for _ in range(NS1)]
        sq = [A.alloc([128, 512], BF16, "sq_%d" % i) for i in range(NS1)]
        Bsq = [Buf() for _ in range(NS1)]
        r1 = [A.alloc([128, 512], F32, "r1_%d" % i) for i in range(NS1)]
        Br1 = [Buf() for _ in range(NS1)]
        NB2 = 2
        RB = os.environ.get("KRB", "act")
        grr_ = [A.alloc([128, 2, 128], F32, "grr%d" % i) for i in range(NB2)]
        Bgrr_ = [Buf() for _ in range(NB2)]
        egr_ = [A.alloc([128, 2, 128], BF16, "egr%d" % i) for i in range(NB2)]
        Begr_ = [Buf() for _ in range(NB2)]
        a1_ = [A.alloc([128, 2, 128], F32, "a1_%d" % k) for k in range(NB2)]
        a2_ = [A.alloc([128, 2, 128], F32, "a2_%d" % k) for k in range(NB2)]
        Ba1_ = [Buf() for _ in range(NB2)]
        Ba2_ = [Buf() for _ in range(NB2)]
        X_ = [[A.alloc([128, 2, 128], neumann_dt, "X%d_%d" % (i, k)) for i in range(2)] for k in range(NB2)]
        XP_ = [[A.alloc([128, 2, 2, 128], neumann_dt, "XP%d_%d" % (i, k)) for i in range(2)] for k in range(NB2)]
        BX_ = [[Buf(), Buf()] for _ in range(NB2)]
        BXP_ = [[Buf(), Buf()] for _ in range(NB2)]
        TT_ = [A.alloc([128, 2, 128], BF16, "TT%d" % i) for i in range(NB2)]
        BTT_ = [Buf() for _ in range(NB2)]
        QKm_ = [A.alloc([128, 2, 128], BF16, "QKm%d" % i) for i in range(NB2)]
        BQKm_ = [Buf() for _ in range(NB2)]
        kb_ = [A.alloc([128, 2, 128], BF16, "kb%d" % i) for i in range(NB2)]
        vb_ = [A.alloc([128, 2, 128], BF16, "vb%d" % i) for i in range(NB2)]
        kdec_ = [A.alloc([128, 2, 128], BF16, "kdec%d" % i) for i in range(NB2)]
        qd_ = [A.alloc([128, 2, 128], BF16, "qd%d" % i) for i in range(NB2)]
        Bkb_, Bvb_, Bkdec_, Bqd_ = [[Buf() for _ in range(NB2)] for _ in range(4)]
        negwT_ = [A.alloc([128, 2, 128], BF16, "negwT%d" % i) for i in range(NB2)]
        BnegwT_ = [Buf() for _ in range(NB2)]
        vnew = A.alloc([128, 2, 128], BF16, "vnew")
        Bvnew = [Buf(), Buf()]
        S32 = A.alloc([128, 2, 128], F32, "S32")
        BS32 = [Buf(), Buf()]
        Sbf = [A.alloc([128, 2, 128], BF16, "Sbf%d" % i) for i in range(2)]
        BSbf = [[Buf(), Buf()], [Buf(), Buf()]]
        identN = C["ident"]
        if neumann_dt != F32:
            identN_t = A.alloc([128, 128], neumann_dt, "identN")
            P.copy("dve", identN_t[:], C["ident"], reads=[Bcst], writes=[Bcst])
            identN = identN_t[:]

        BpKK, BpW, BpGR, BpT, BpA, BpB, BpV, BpO = Bps
        BpS = BpV
        psKK = psum[0][:, 0:256]
        psW = psum[1][:, 0:256].rearrange("p (h n) -> p h n", h=2)
        psGR = psum[2][:, 0:256].rearrange("p (h n) -> p h n", h=2)
        psT = psum[3][:, 0:256].rearrange("p (h n) -> p h n", h=2)
        psA = psum[4][:].rearrange("p (h n) -> p h n", h=2)
        psB = psum[5][:, 0:256].rearrange("p (h n) -> p h n", h=2)
        psV = psum[6][:, 0:256].rearrange("p (h n) -> p h n", h=2)
        psS = psum[6][:, 256:512].rearrange("p (h n) -> p h n", h=2)
        psO = psum[7][:, 0:256].rearrange("p (h n) -> p h n", h=2)
        evq = [0]

        def ev_eng():
            evq[0] += 1
            return "act" if evq[0] % 2 else "dve"

        for hq in range(8):
            cols = [(hq * 128, 128, 0), (1024 + hq * 128, 128, 128), (2048 + hq * 256, 256, 256), (4096 + hq * 256, 256, 512)]
            gp = hq % 2
            qkT, BqkT, ktok, Bktok = qkT_g[gp], BqkT_g[gp], ktok_g[gp], Bktok_g[gp]
            vtok, Bvtok, zs, Bzs = vtok_g[gp], Bvtok_g[gp], zs_g[gp], Bzs_g[gp]
            ogun, Bog, ssq, Bssq = ogun_g[gp], Bog_g[gp], ssq_g[gp], Bssq_g[gp]
            wbf, Bwbf = wbf_[hq % 2], Bwbf_[hq % 2]
            for pi_ in range(2):
                P.op("pool", lambda e, pi_=pi_: e.memset(pre_[pi_][:, 0:3], 0.0), writes=[Bpre_[pi_]])
            prec = [0]
            chunks = [hq, 8 + hq, 16 + 2 * hq, 17 + 2 * hq]
            for ci, ch in enumerate(chunks):
                for j in range(4):
                    P.ts("dve", dg[:, ci, j, :], C["ident"], cw[:, ch, j:j + 1], None, ALU.mult, reads=[Bcst, Bv], writes=[Bdg])

            def inproj_fm(wc0):
                pre, Bpre = pre_[prec[0] % 2], Bpre_[prec[0] % 2]
                prec[0] += 1
                for tb in range(4):
                    pb = (1, 3)[tb % 2]
                    for kc in range(8):
                        P.mm(psum[pb][:, :], wbf[:, kc, wc0:wc0 + 128], hT[:, kc, tb * 512:(tb + 1) * 512],
                             start=(kc == 0), stop=(kc == 7), reads=[Bwbf, BhT], writes=[Bps[pb]])
                    P.copy(ev_eng(), pre[:, 3 + tb * 512:3 + (tb + 1) * 512], psum[pb][:, :], reads=[Bps[pb]], writes=[Bpre])
                return pre, Bpre

            for f in range(2):
                pre, Bpre = inproj_fm(f * 128)
                for tb in range(4):
                    i = (f * 4 + tb) % NS1
                    pb = (0, 2)[tb % 2]
                    for j in range(4):
                        P.mm(psum[pb][:, :], dg[:, f, j, :], pre[:, tb * 512 + j:tb * 512 + j + 512], start=(j == 0), stop=(j == 3),
                             reads=[Bdg, Bpre], writes=[Bps[pb]])
                    P.actv(s1[i][:], psum[pb][:, :], AF.Silu, reads=[Bps[pb]], writes=[Bs1[i]])
                    P.tt("pool", sq[i][:], s1[i][:], s1[i][:], ALU.mult, reads=[Bs1[i]], writes=[Bsq[i]])
                    P.mm(psum[pb][:, :], onesb[:], sq[i][:], reads=[Bsq[i], Bcst], writes=[Bps[pb]])
                    P.actv(r1[i][:], psum[pb][:, :], AF.Sqrt, reads=[Bps[pb]], writes=[Br1[i]], bias=EPS)
                    P.op("dve", lambda e, i=i: e.reciprocal(out=r1[i][:], in_=r1[i][:]), reads=[Br1[i]], writes=[Br1[i]])
                    sc = (128.0 ** -0.5) if f == 0 else 1.0
                    P.stt("dve", qkT[:, f, tb * 512:(tb + 1) * 512], s1[i][:], sc, r1[i][:], ALU.mult, ALU.mult,
                          reads=[Bs1[i], Br1[i]], writes=[BqkT])
            for t in range(NT):
                pb = (0, 2)[(t // 4) % 2]
                pst = psum[pb][:].bitcast(BF16)
                P.tr(pst[:, (t % 4) * 128:(t % 4 + 1) * 128], qkT[:, 1, t * 128:(t + 1) * 128], identb[:], reads=[BqkT, Bcst], writes=[Bps[pb]])
                if t % 4 == 3:
                    t0 = t - 3
                    P.copy(ev_eng(), ktok[:, t0:t0 + 4, :], pst[:, 0:512].rearrange("p (a b) -> p a b", a=4), reads=[Bps[pb]], writes=[Bktok])
            for vh in range(2):
                pre, Bpre = inproj_fm(256 + vh * 128)
                for t in range(NT):
                    pb = (0, 2)[(t // 4) % 2]
                    for j in range(4):
                        P.mm(psum[pb][:, (t % 4) * 128:(t % 4 + 1) * 128], pre[:, t * 128 + j:t * 128 + j + 128], dg[:, 2 + vh, j, :],
                             start=(j == 0), stop=(j == 3), reads=[Bdg, Bpre], writes=[Bps[pb]])
                    if t % 4 == 3:
                        t0 = t - 3
                        P.actv(vtok[:, t0:t0 + 4, vh, :], psum[pb][:, :].rearrange("p (a b) -> p a b", a=4), AF.Silu,
                               reads=[Bps[pb]], writes=[Bvtok])
            for t in range(NT):
                pb = (1, 3)[t % 2]
                for kc in range(8):
                    P.mm(psum[pb][:, 0:256], hT[:, kc, t * 128:(t + 1) * 128], wbf[:, kc, 512:768], start=(kc == 0), stop=(kc == 7),
                         reads=[Bwbf, BhT], writes=[Bps[pb]])
                i = t % NS1
                P.actv(s1[i][:, 0:256], psum[pb][:, 0:256], AF.Silu, reads=[Bps[pb]], writes=[Bs1[i]])
                P.tt("pool", zs[:, t, :, :], s1[i][:, 0:256].rearrange("p (a b) -> p a b", a=2),
                     bc(normw_bc[:].unsqueeze(1), [128, 2, 128]), ALU.mult, reads=[Bs1[i], Bv], writes=[Bzs])

            if hq == 0:
                tap("qkT", qkT[:], [BqkT]); tap("ktok", ktok[:], [Bktok]); tap("vtok", vtok[:], [Bvtok]); tap("zs", zs[:], [Bzs])
            ck("C1")
            if hq + 1 < 8:
                load_gdn_w(hq + 1)
            for hl in range(2):
                P.op("pool", lambda e, hl=hl: e.memset(S32[:, hl, :], 0.0), writes=[BS32[hl]])
                P.op("pool", lambda e, hl=hl: e.memset(Sbf[0][:, hl, :], 0.0), writes=[BSbf[0][hl]])
            sp = 0
            for t in range(NT):
                tl = slice(t * 128, (t + 1) * 128)
                h0 = 2 * hq
                k2 = t % NB2
                grr, Bgrr, egr, Begr = grr_[k2], Bgrr_[k2], egr_[k2], Begr_[k2]
                a1, a2, Ba1, Ba2 = a1_[k2], a2_[k2], Ba1_[k2], Ba2_[k2]
                TT, BTT, QKm, BQKm = TT_[k2], BTT_[k2], QKm_[k2], BQKm_[k2]
                kb, vb, kdec, qd = kb_[k2], vb_[k2], kdec_[k2], qd_[k2]
                Bkb, Bvb, Bkdec, Bqd = Bkb_[k2], Bvb_[k2], Bkdec_[k2], Bqd_[k2]
                negwT, BnegwT = negwT_[k2], BnegwT_[k2]
                X, XP, BX, BXP = X_[k2], XP_[k2], BX_[k2], BXP_[k2]
                P.tt("dve", grr[:], bc(C["ident"].unsqueeze(1), [128, 2, 128]),
                     bc(sca[:, GC, t, h0:h0 + 2].unsqueeze(2), [128, 2, 128]), ALU.mult, reads=[Bcst, Bsc], writes=[Bgrr])
                P.mm(psum[2][:, 0:256], C["ones"], grr[:].rearrange("p a b -> p (a b)"), reads=[Bcst, Bgrr], writes=[BpGR])
                P.actv(egr[:], psGR, AF.Exp, reads=[BpGR], writes=[Begr])
                P.mm(psKK[:, 0:128], qkT[:, 1, tl], qkT[:, 1, tl], reads=[BqkT], writes=[BpKK])
                P.mm(psKK[:, 128:256], qkT[:, 1, tl], qkT[:, 0, tl], reads=[BqkT], writes=[BpKK])
                ck("C2a")
                cur = 0
                for hl in range(2):
                    h = h0 + hl
                    P.stt("dve", a1[:, hl, :], psGR[:, hl, :], sca[:, GC, t, h:h + 1], C["mask_a"], ALU.subtract, ALU.max,
                          reads=[BpGR, Bsc, Bcst], writes=[Ba1])
                    P.stt("dve", a2[:, hl, :], psGR[:, hl, :], sca[:, GC, t, h:h + 1], C["mask_q"], ALU.subtract, ALU.min,
                          reads=[BpGR, Bsc, Bcst], writes=[Ba2])
                P.actv(a1[:], a1[:], AF.Exp, reads=[Ba1], writes=[Ba1], scale=-1.0)
                P.actv(a2[:], a2[:], AF.Exp, reads=[Ba2], writes=[Ba2])
                for hl in range(2):
                    h = h0 + hl
                    P.stt("dve", X[cur][:, hl, :], psKK[:, 0:128], sca[:, NBETA, t, h:h + 1], a1[:, hl, :], ALU.mult, ALU.mult,
                          reads=[BpKK, Bsc, Ba1], writes=[BX[cur]])
                    P.tt("dve", QKm[:, hl, :], psKK[:, 128:256], a2[:, hl, :], ALU.mult, reads=[BpKK, Ba2], writes=[BQKm])
                    P.tr(psT[:, hl, :], X[cur][:, hl, :], identN, reads=[BX[cur], Bcst], writes=[BpT])
                ck("D4")
                P.copy("act", XP[cur][:, :, 0, :], psT, reads=[BpT], writes=[BXP[cur]])
                ck("D5")
                P.copy("dve", XP[cur][:, :, 1, :], bc(identN.unsqueeze(1), [128, 2, 128]), reads=[Bcst], writes=[BXP[cur]])
                ck("C2b")
                for hl in range(2):
                    h = h0 + hl
                    P.ts("act", kb[:, hl, :], ktok[:, t, :], sca[:, BK, t, h:h + 1], None, ALU.mult, reads=[Bktok, Bsc], writes=[Bkb])
                    P.ts("act", vb[:, hl, :], vtok[:, t, hl, :], sca[:, BETA, t, h:h + 1], None, ALU.mult, reads=[Bvtok, Bsc], writes=[Bvb])
                    P.ts("pool", kdec[:, hl, :], ktok[:, t, :], sca[:, EKD, t, h:h + 1], None, ALU.mult, reads=[Bktok, Bsc], writes=[Bkdec])
                P.tt("pool", qd[:], bc(qkT[:, 0, tl].unsqueeze(1), [128, 2, 128]), egr[:], ALU.mult, reads=[BqkT, Begr], writes=[Bqd])
                ck("C2c")
                for k in range(6):
                    nxt = 1 - cur
                    last = (k == 5)
                    for hl in range(2):
                        if not last:
                            P.mm(psA[:, hl, :], X[cur][:, hl, :], XP[cur][:, hl, :, :].rearrange("p a b -> p (a b)"),
                                 reads=[BX[cur], BXP[cur]], writes=[BpA])
                            P.mm(psB[:, hl, :], XP[cur][:, hl, 0, :], X[cur][:, hl, :], reads=[BX[cur], BXP[cur]], writes=[BpB])
                        else:
                            P.mm(psA[:, hl, 128:256], X[cur][:, hl, :], XP[cur][:, hl, 1, :], reads=[BX[cur], BXP[cur]], writes=[BpA])
                    if not last:
                        P.copy("act", XP[nxt][:, :, 0, :], psA[:, :, 0:128], reads=[BpA], writes=[BXP[nxt]])
                        P.tt("dve", XP[nxt][:, :, 1, :], XP[cur][:, :, 1, :], psA[:, :, 128:256], ALU.add, reads=[BpA, BXP[cur]], writes=[BXP[nxt]])
                        P.copy("dve" if k % 2 == 0 else "act", X[nxt][:], psB, reads=[BpB], writes=[BX[nxt]])
                    else:
                        P.tt("dve", TT[:], XP[cur][:, :, 1, :], psA[:, :, 128:256], ALU.add, reads=[BpA, BXP[cur]], writes=[BTT])
                    cur = nxt
                ck("C2d")
                if hq == 0 and t == 0:
                    tap("TT", TT[:], [BTT]); tap("QKm", QKm[:], [BQKm]); tap("a1", a1[:, 0, :], [Ba1]); tap("a2", a2[:, 0, :], [Ba2])
                for hl in range(2):
                    P.mm(psW[:, hl, :], kb[:, hl, :], TT[:, hl, :], reads=[Bkb, BTT], writes=[BpW])
                P.actv(negwT[:], psW, AF.Copy, reads=[BpW], writes=[BnegwT], scale=-1.0)
                ck("C2e")
                for cb in range(2):
                    rows = slice(cb * 64, cb * 64 + 64)
                    tp = (0, 64 * cb)
                    tpk = (64 * cb, 64 * cb)
                    for hl in range(2):
                        h = h0 + hl
                        P.mm(psV[rows, hl, :], TT[:, hl, rows], vb[:, hl, :], start=True, stop=False,
                             reads=[BTT, Bvb], writes=[BpV], tile_position=tp)
                        P.mm(psV[rows, hl, :], negwT[:, hl, rows], Sbf[sp][:, hl, :], start=False, stop=True,
                             reads=[BnegwT, BSbf[sp][hl]], writes=[BpV], tile_position=tp)
                    P.copy("act" if cb == 0 else "dve", vnew[rows, :, :], psV[rows, :, :], reads=[BpV], writes=[Bvnew[0], Bvnew[1]])
                    for hl in range(2):
                        P.mm(psS[:, hl, :], kdec[rows, hl, :], vnew[rows, hl, :], reads=[Bkdec, Bvnew[hl]], writes=[BpS],
                             tile_position=(64 * cb, 0))
                    for hl in range(2):
                        P.mm(psO[rows, hl, :], qd[:, hl, rows], Sbf[sp][:, hl, :], start=True, stop=False,
                             reads=[Bqd, BSbf[sp][hl]], writes=[BpO], tile_position=tp)
                        P.mm(psO[rows, hl, :], QKm[rows, hl, rows], vnew[rows, hl, :], start=False, stop=True,
                             reads=[BQKm, Bvnew[hl]], writes=[BpO], tile_position=tpk)
                    for hl in range(2):
                        h = h0 + hl
                        eg = sca[:, GLA if cb == 0 else GLB, t, h:h + 1]
                        P.stt("dve", Sbf[1 - sp][:, hl, :], S32[:, hl, :], eg, psS[:, hl, :], ALU.mult, ALU.add,
                              reads=[BS32[hl], Bsc, BpS], writes=[BSbf[1 - sp][hl]])
                        P.stt("dve", S32[:, hl, :], S32[:, hl, :], eg, psS[:, hl, :], ALU.mult, ALU.add,
                              reads=[BS32[hl], Bsc, BpS], writes=[BS32[hl]])
                    sp = 1 - sp
                    ck("C2f%d" % cb)
                for hl in range(2):
                    P.actv(a1[:, hl, :], psO[:, hl, :], AF.Square, reads=[BpO], writes=[Ba1, Bssq], accum_out=ssq[:, t, hl:hl + 1])
                P.tt("dve", ogun[:, t, :, :], psO, zs[:, t, :, :], ALU.mult, reads=[BpO, Bzs], writes=[Bog])
                ck("C2")
            if hq == 0:
                tap("ogun", ogun[:], [Bog]); tap("ssq", ssq[:], [Bssq])
            ssf = ssq[:].rearrange("p t h -> p (t h)")
            P.actv(ssf, ssf, AF.Sqrt, reads=[Bssq], writes=[Bssq], scale=1.0 / 128, bias=EPS)
            P.op("dve", lambda e, ssf=ssf: e.reciprocal(out=ssf, in_=ssf), reads=[Bssq], writes=[Bssq])
            P.tt("pool", ogun[:].rearrange("p t h e -> p (t h) e"), ogun[:].rearrange("p t h e -> p (t h) e"),
                 bc(ssf.unsqueeze(2), [128, 2 * NT, 128]), ALU.mult, reads=[Bog, Bssq], writes=[Bog])
            P.dma(og_d.rearrange("(t p) (h e) -> p t h e", p=128, e=128)[:, :, 2 * hq:2 * hq + 2, :], ogun[:], reads=[Bog], q="sp")
            if hq == 7:
                wsrc = gdn_w_out.rearrange("(h e) n -> e h n", e=128)
                for hp in range(4):
                    P.dma(wo_alias[:, hp * 4:(hp + 1) * 4, :], wsrc[:, hp * 4:(hp + 1) * 4, :], writes=[BhT], q="pool")
            ck("C3")
        A.release(mG)
        ck("C4")

        if mode == "all":
            make_gate_bc(0)
            nsa_consts()
        mO = A.mark()
        wo, Bwo = wo_alias, BhT
        NO = 4
        xr = [A.alloc([128, D], F32, "xr%d" % i) for i in range(NO)]
        Bxr = [Buf() for _ in range(NO)]
        yo = [A.alloc([128, D], F32, "yo%d" % i) for i in range(NO)]
        Byo = [Buf() for _ in range(NO)]
        ogt = [A.alloc([128, 2048], BF16, "ogt%d" % i) for i in range(NO)]
        Bogt = [Buf() for _ in range(NO)]
        oTt = [A.alloc([128, 16, 128], BF16, "oTt%d" % i) for i in range(NO)]
        BoTt = [Buf() for _ in range(NO)]
        for t in range(NT):
            i = t % NO
            P.dma(xr[i][:], src_d[t * 128:(t + 1) * 128, :], writes=[Bxr[i]], q="sp")
            P.dma(ogt[i][:], og_d[t * 128:(t + 1) * 128, :], writes=[Bogt[i]], q="sp")
            for half in range(2):
                pb = 2 + half
                pst = psum[pb][:].bitcast(BF16)
                for h8 in range(8):
                    h = half * 8 + h8
                    P.tr(pst[:, h8 * 128:(h8 + 1) * 128], ogt[i][:, h * 128:(h + 1) * 128], identb[:], reads=[Bogt[i], Bcst], writes=[Bps[pb]])
                P.copy("act" if half == 0 else "dve", oTt[i][:, half * 8:(half + 1) * 8, :].rearrange("p a b -> p (a b)"), pst[:, 0:1024],
                       reads=[Bps[pb]], writes=[BoTt[i]])
            for hh in range(2):
                pb = hh
                for h in range(16):
                    P.mm(psum[pb][:, :], oTt[i][:, h, :], wo[:, h, hh * 512:(hh + 1) * 512],
                         start=(h == 0), stop=(h == 15), reads=[BoTt[i], Bwo], writes=[Bps[pb]])
                P.tt("dve", yo[i][:, hh * 512:(hh + 1) * 512], psum[pb][:, :], gate_bc[:, hh * 512:(hh + 1) * 512], ALU.mult,
                     reads=[Bps[pb], Bmod], writes=[Byo[i]])
            P.tt("pool", yo[i][:], yo[i][:], xr[i][:], ALU.add, reads=[Byo[i], Bxr[i]], writes=[Byo[i]])
            o = P.dma(dst_d[t * 128:(t + 1) * 128, :], yo[i][:], reads=[Byo[i]], q="sp")
            final_ops.append(o)
        A.release(mO)
        A.release(mL)

    NS = {}

    def nsa_consts():
        NS["top0"] = A.top
        kcmpT = A.alloc_top([128, 4, 128], BF16, "kcmpT")
        vcmp = A.alloc_top([128, 4, 97], F32, "vcmp")
        bias = A.alloc_top([128, 2, 16, 128], BF16, "bias")
        gsm = A.alloc_top([128, 16, 128], BF16, "gsm")
        shb = A.alloc_top([128, NT, 127], BF16, "shb")
        emb = A.alloc_top([128, NT, 128], BF16, "emb")
        wmb = A.alloc_top([128, 128], BF16, "wmb")
        cmask = A.alloc_top([128, NT, 32], F32, "cmask")
        fbias = A.alloc_top([128, NT, 32], F32, "fbias")
        fgb = A.alloc_top([128, D], F32, "fgb")
        Bkc, Bvc, Bbias, Bgsm, Bk1 = [Buf() for _ in range(5)]
        P.dma(cmask[:], nsa_cmask, writes=[Bk1])
        P.dma(fbias[:], nsa_fbias, writes=[Bk1])
        P.dma(fgb[:], AP(final_g.tensor, 0, [[0, 128], [1, D]]), writes=[Bk1])
        P.op("pool", lambda e: e.memset(vcmp[:], 1.0), writes=[Bvc])
        P.dma(vcmp[0:127, :, 65:97], AP(nsa_ovl.tensor, 0, [[32, 127], [0, 4], [1, 32]]), writes=[Bvc], q="pool")
        relb = A.alloc([33, 16], F32, "relb")
        ohs = A.alloc([33, 384], F32, "ohs")
        tbl = A.alloc([16, 384], F32, "tbl")
        Bt0, Bt1, BZ = Buf(), Buf(), Buf()
        P.op("pool", lambda e: e.memset(relb[:], 1.0), writes=[Bt0])
        P.dma(relb[0:32, :], rel_bias_d, writes=[Bt0])
        P.dma(ohs[:], nsa_oh, writes=[Bt0])
        P.mm(psum[3][0:16, 0:384], relb[:, :], ohs[:, :], reads=[Bt0], writes=[Bps[3]])
        P.copy("dve", tbl[:], psum[3][0:16, 0:384], reads=[Bps[3]], writes=[Bt1])
        WD = 400
        P.dma(AP(zt_d.tensor, 0, [[128 * WD, 16], [WD, 128], [1, 384]]), bc(tbl[:].unsqueeze(1), [16, 128, 384]),
              reads=[Bt1], writes=[BZ])
        P.op("pool", lambda e: e.memset(gsm[:], 0.0), writes=[Bgsm])
        P.op("pool", lambda e: e.memset(gsm[0:16, :, :], -BIG), writes=[Bgsm])
        for h in range(16):
            for dl in range(2):
                P.dma(bias[:, dl, h, :], AP(zt_d.tensor, h * 128 * WD + 128 * (dl + 1), [[WD - 1, 128], [1, 128]]),
                      reads=[BZ], writes=[Bbias], q="pool")
            P.dma(gsm[0:15, h, :], AP(zt_d.tensor, h * 128 * WD + 225, [[WD - 16, 15], [1, 128]]), reads=[BZ], writes=[Bgsm], q="pool")
        P.dma(shb[:], nsa_sh, writes=[Bk1], q="pool")
        P.dma(emb[:], nsa_em, writes=[Bk1], q="pool")
        P.dma(wmb[:], nsa_wm, writes=[Bk1], q="pool")
        NS.update(kcmpT=kcmpT, vcmp=vcmp, bias=bias, gsm=gsm, shb=shb, emb=emb, wmb=wmb, cmask=cmask, fbias=fbias, fgb=fgb,
                  Bkc=Bkc, Bvc=Bvc, Bbias=Bbias, Bgsm=Bgsm, Bk1=Bk1)

    def layer_nsa(src_d, dst_d):
        l = 1
        mL = A.mark()
        if not NS:
            nsa_consts()
            A.release(mL)
        top0 = NS["top0"]
        kcmpT, vcmp, bias, gsm, shb, emb, wmb = NS["kcmpT"], NS["vcmp"], NS["bias"], NS["gsm"], NS["shb"], NS["emb"], NS["wmb"]
        cmask, fbias, fgb = NS["cmask"], NS["fbias"], NS["fgb"]
        Bkc, Bvc, Bbias, Bgsm, Bk1 = NS["Bkc"], NS["Bvc"], NS["Bbias"], NS["Bgsm"], NS["Bk1"]
        BqT, BksT, BkwT, Bvs, Bvw, Bzsn, Bgts = [Buf() for _ in range(7)]
        ck("N0")

        mP = A.mark()
        hT = A.alloc([128, 8, T], BF16, "hT1")
        BhT = Buf("hT1")
        wbf = [A.alloc([128, 8, 512], BF16, "nwbf%d" % i) for i in range(2)]
        Bwbf = [Buf(), Buf()]
        wcnt = [0]
        evq = [0]

        def ev_eng():
            evq[0] += 1
            return "act" if evq[0] % 2 else "dve"

        def load_w(c0, n):
            i = wcnt[0] % 2
            wcnt[0] += 1
            for kh in range(2):
                P.dma(wbf[i][:, kh * 4:(kh + 1) * 4, 0:n], nsa_w[kh * 512:(kh + 1) * 512, c0:c0 + n].rearrange("(kc p) n -> p kc n", p=128),
                      writes=[Bwbf[i]], q="pool")
            return wbf[i], Bwbf[i]

        pre_w = [load_w(2048, 512), load_w(0, 512)]
        run_stage_a([0, 1])
        make_hT(src_d, l, hT, BhT)
        A.off = stage_a_mark[0] if stage_a_mark[0] is not None else A.off
        make_gate_bc(l)
        ck("N0b")

        pcnt = [0]

        def proj_fm(w, Bw, wc0, dst, Bdst, scale=None):
            for tb in range(4):
                pb = pcnt[0] % 2
                pcnt[0] += 1
                for kc in range(8):
                    P.mm(psum[pb][:, :], w[:, kc, wc0:wc0 + 128], hT[:, kc, tb * 512:(tb + 1) * 512],
                         start=(kc == 0), stop=(kc == 7), reads=[Bw, BhT], writes=[Bps[pb]])
                e = ev_eng()
                if scale is None:
                    P.copy(e, dst[:, tb * 512:(tb + 1) * 512], psum[pb][:, :], reads=[Bps[pb]], writes=[Bdst])
                elif e == "act":
                    P.actv(dst[:, tb * 512:(tb + 1) * 512], psum[pb][:, :], AF.Copy, reads=[Bps[pb]], writes=[Bdst], scale=scale)
                else:
                    P.ts("dve", dst[:, tb * 512:(tb + 1) * 512], psum[pb][:, :], scale, None, ALU.mult, reads=[Bps[pb]], writes=[Bdst])

        CQ, CKS, CKW, CKC, CVC, CVSW, CZ, CG = 0, 1024, 1536, 2048, 2304, 2560, 3072, 4096

        mC = A.mark()
        kvcT = A.alloc([128, 4, T], BF16, "kvcT")
        BkvcT = Buf()
        w, Bw = pre_w[0]
        for c in range(4):
            proj_fm(w, Bw, c * 128, kvcT[:, c, :], BkvcT)
        w1b = A.alloc([128, 32, 64], BF16, "w1b")
        w2b = A.alloc([64, 128], BF16, "w2b")
        posb = A.alloc([128, 32], BF16, "posb")
        c1 = A.alloc([64, 1], F32, "c1")
        hid = A.alloc([64, 4, 128], BF16, "hid")
        Bw1, Bw2, Bpos, Bc1, Bhid = Buf(), Buf(), Buf(), Buf(), Buf()
        for br in range(2):
            for half in range(2):
                P.dma(w1b[half * 64:(half + 1) * 64, :, :], nsa_w1[br].rearrange("(l d) j -> d l j", d=64), writes=[Bw1], q="pool")
                P.dma(posb[half * 64:(half + 1) * 64, :], nsa_pos[br].rearrange("l d -> d l"), writes=[Bpos], q="pool",
                      allow_slow_non_contiguous=True)
            P.dma(w2b[:, 0:64], nsa_w2[br], writes=[Bw2], q="pool")
            P.dma(w2b[:, 64:128], nsa_w2[br], writes=[Bw2], q="pool")
            for lq in range(32):
                P.mm(psum[2][0:64, 0:1], w1b[0:64, lq, :], posb[0:64, lq:lq + 1], start=(lq == 0), stop=(lq == 31),
                     reads=[Bw1, Bpos], writes=[Bps[2]])
            P.copy("dve", c1[:], psum[2][0:64, 0:1], reads=[Bps[2]], writes=[Bc1])
            for g in range(4):
                half = g % 2
                rows = slice(half * 64, half * 64 + 64)
                src = kvcT[rows, br * 2 + g // 2, :]
                for lq in range(32):
                    P.mm(psum[3][0:64, 0:127], w1b[rows, lq, :], src[:, lq:lq + 16 * 126 + 1:16], start=(lq == 0), stop=(lq == 31),
                         reads=[Bw1, BkvcT], writes=[Bps[3]], tile_position=(half * 64, 0))
                P.actv(hid[:, g, 0:127], psum[3][0:64, 0:127], AF.Silu, reads=[Bps[3], Bc1], writes=[Bhid], bias=c1[:, 0:1])
                if br == 0:
                    P.mm(psum[2][:, 0:127], w2b[:, :], hid[:, g, 0:127], reads=[Bw2, Bhid], writes=[Bps[2]])
                    P.copy("dve", kcmpT[:, g, 0:127], psum[2][:, 0:127], reads=[Bps[2]], writes=[Bkc])
                else:
                    P.mm(psum[2][0:127, 0:64], hid[:, g, 0:127], w2b[:, 0:64], reads=[Bw2, Bhid], writes=[Bps[2]])
                    P.copy("dve", vcmp[0:127, g, 0:64], psum[2][0:127, 0:64], reads=[Bps[2]], writes=[Bvc])
        A.release(mC)
        ck("N1")
        qT = A.alloc_top([128, 8, T], BF16, "qT")
        ksT = A.alloc_top([128, 4, T], BF16, "ksT")
        kwT = A.alloc_top([128, 4, T], BF16, "kwT")
        vs_aug = A.alloc_top([128, NT, 4, 65], BF16, "vs_aug")
        vw_aug = A.alloc_top([128, NT, 4, 65], BF16, "vw_aug")
        zsn = A.alloc_top([128, NT, D], BF16, "zsn")
        gts = A.alloc_top([128, NT, 48], F32, "gts")
        P.op("pool", lambda e: e.memset(vs_aug[:], 1.0), writes=[Bvs])
        P.op("pool", lambda e: e.memset(vw_aug[:], 1.0), writes=[Bvw])

        for half2 in range(2):
            w, Bw = pre_w[1] if half2 == 0 else load_w(CQ + half2 * 512, 512)
            for c in range(4):
                proj_fm(w, Bw, c * 128, qT[:, half2 * 4 + c, :], BqT, scale=0.125)
        w, Bw = load_w(CKS, 512)
        for g in range(4):
            proj_fm(w, Bw, g * 128, ksT[:, g, :], BksT)
        w, Bw = load_w(CKW, 512)
        for g in range(4):
            proj_fm(w, Bw, g * 128, kwT[:, g, :], BkwT)
        w, Bw = load_w(CVSW, 512)
        for t in range(NT):
            pb = pcnt[0] % 2
            pcnt[0] += 1
            for kc in range(8):
                P.mm(psum[pb][:, :], hT[:, kc, t * 128:(t + 1) * 128], w[:, kc, :], start=(kc == 0), stop=(kc == 7),
                     reads=[Bw, BhT], writes=[Bps[pb]])
            P.copy("act", vs_aug[:, t, :, 0:64], psum[pb][:, 0:256].rearrange("p (g d) -> p g d", g=4), reads=[Bps[pb]], writes=[Bvs])
            P.copy("dve", vw_aug[:, t, :, 0:64], psum[pb][:, 256:512].rearrange("p (g d) -> p g d", g=4), reads=[Bps[pb]], writes=[Bvw])
        for zh in range(2):
            w, Bw = load_w(CZ + zh * 512, 512)
            for t in range(NT):
                pb = pcnt[0] % 2
                pcnt[0] += 1
                for kc in range(8):
                    P.mm(psum[pb][:, :], hT[:, kc, t * 128:(t + 1) * 128], w[:, kc, :], start=(kc == 0), stop=(kc == 7),
                         reads=[Bw, BhT], writes=[Bps[pb]])
                P.actv(zsn[:, t, zh * 512:(zh + 1) * 512], psum[pb][:, :], AF.Silu, reads=[Bps[pb]], writes=[Bzsn])
        w, Bw = load_w(CG, 48)
        for t in range(NT):
            pb = pcnt[0] % 2
            pcnt[0] += 1
            for kc in range(8):
                P.mm(psum[pb][:, 0:48], hT[:, kc, t * 128:(t + 1) * 128], w[:, kc, 0:48], start=(kc == 0), stop=(kc == 7),
                     reads=[Bw, BhT], writes=[Bps[pb]])
            P.copy("dve", gts[:, t, :], psum[pb][:, 0:48], reads=[Bps[pb]], writes=[Bgts])
        P.actv(gts[:], gts[:], AF.Sigmoid, reads=[Bgts], writes=[Bgts])
        A.release(mP)
        ck("N2")

        wo = A.alloc_top([128, 8, D], BF16, "nwo")
        Bwo = Buf()
        for ch in range(2):
            P.dma(wo[:, ch * 4:(ch + 1) * 4, :], nsa_wo[ch * 512:(ch + 1) * 512, :].rearrange("(c p) n -> p c n", p=128), writes=[Bwo], q="pool")
        NPT = 3
        PT = [A.alloc([128, 512], BF16, "PT%d" % i) for i in range(NPT)]
        BPT = [Buf() for _ in range(NPT)]
        ptc = [0]
        PTf = A.alloc([128, 2, 512], F32, "PTf")
        BPTf = [Buf() for _ in range(2)]
        accc = A.alloc([128, 4, 4, 97], F32, "accc")
        Baccc = Buf()
        accw = A.alloc([128, 4, 4, 65], F32, "accw")
        Baccw = [Buf() for _ in range(4)]
        accs = [A.alloc([128, 4, 65], F32, "accs%d" % i) for i in range(2)]
        Baccs = [Buf(), Buf()]
        imp = A.alloc([128, 4, 32], F32, "imp")
        mx8 = A.alloc([128, 4, 8], F32, "mx8")
        selb = A.alloc([128, 4, 32], BF16, "selb")
        selbT = A.alloc([128, 4, 128], BF16, "selbT")
        Bimp, Bselb, BselbT = Buf(), Buf(), [Buf() for _ in range(4)]
        P.op("pool", lambda e: e.memset(selbT[:], 0.0), writes=BselbT)
        rec = A.alloc([128, 3, 16], F32, "rec")
        Brec = Buf()
        recsw = A.alloc([128, 2, 4], F32, "recsw")
        Brecsw = Buf()
        tmpo = A.alloc([128, 2, 4, 64], F32, "tmpo")
        Btmpo = Buf()
        og = A.alloc([128, D], BF16, "og")
        Bog = Buf()
        ogT = A.alloc([128, 8, 128], BF16, "ogT")
        BogT = Buf()
        yo = A.alloc([128, D], F32, "nyo")
        Byo = Buf()
        fst = A.alloc([128, NT, 3], F32, "fst")
        Bfst = Buf()
        junk = tmpo[:].rearrange("p a b c -> p (a b c)").bitcast(BF16)
        impt = tmpo[:].rearrange("p a b c -> p (a b c)").rearrange("p (g b s) -> p g b s", g=4, b=4)
        scnt = [0]
        TINY = 1e-30

        qpad = [A.alloc([128, 512], BF16, "qpad%d" % g) for g in range(4)]
        Bqpad = [Buf() for _ in range(4)]
        for g in range(4):
            P.op("pool", lambda e, g=g: e.memset(qpad[g][:], 0.0), writes=[Bqpad[g]])

        def build_qpad(i, g):
            qs = slice(i * 128, (i + 1) * 128)
            P.copy("pool", qpad[g][0:64, 0:256].rearrange("p (a b) -> p a b", a=2), qT[0:64, 2 * g:2 * g + 2, qs], reads=[BqT], writes=[Bqpad[g]])
            P.copy("dve", qpad[g][64:128, 256:512].rearrange("p (a b) -> p a b", a=2), qT[64:128, 2 * g:2 * g + 2, qs], reads=[BqT], writes=[Bqpad[g]])

        def scores(i, g, kT_t, Bk, ncols_k, kslice, extra):
            pb = scnt[0] % 4
            scnt[0] += 1
            P.mm(psum[pb][0:ncols_k, 0:512], kT_t[:, g, kslice], qpad[g][:, :], start=True, stop=(len(extra) == 0),
                 reads=[Bk, Bqpad[g]], writes=[Bps[pb]])
            for n, (lt, rh, rd) in enumerate(extra):
                P.mm(psum[pb][0:ncols_k, 0:512], lt, rh, start=False, stop=(n == len(extra) - 1), reads=rd, writes=[Bps[pb]])
            return pb

        def branch_tiles(i, g, br):
            kT_t, Bk, v_aug, Bv = (ksT, BksT, vs_aug, Bvs) if br == 1 else (kwT, BkwT, vw_aug, Bvw)
            bank = 6 if br == 1 else 5
            j0 = 0 if br == 1 else max(0, i - 4)
            first = True
            for j in range(j0, i + 1):
                dl = i - j
                extra = []
                if br == 1 and j != i and i >= 4:
                    extra.append((emb[:, j, :], bc(selbT[:, g, :].unsqueeze(1), [128, 4, 128]), [Bk1, BselbT[g]]))
                if dl <= 1:
                    extra.append((identb[:], bias[:, dl, 4 * g:4 * g + 4, :].rearrange("p a b -> p (a b)"), [Bcst, Bbias]))
                if br == 2 and dl == 4:
                    extra.append((identb[:], bc(wmb[:].unsqueeze(1), [128, 4, 128]), [Bcst, Bk1]))
                pb = scores(i, g, kT_t, Bk, 128, slice(j * 128, (j + 1) * 128), extra)
                pi = ptc[0] % NPT
                ptc[0] += 1
                P.actv(PT[pi][:], psum[pb][:, :], AF.Exp, reads=[Bps[pb]], writes=[BPT[pi]])
                for b in range(4):
                    P.mm(psum[bank][:, b * 65:(b + 1) * 65], PT[pi][:, b * 128:(b + 1) * 128], v_aug[:, j, g, :],
                         start=first, stop=(j == i), reads=[BPT[pi], Bv], writes=[Bps[bank]], skip_group_check=True)
                    first = False
            if br == 1:
                P.copy("act", accs[g % 2][:], psum[bank][:, 0:260].rearrange("p (a b) -> p a b", a=4), reads=[Bps[bank]], writes=[Baccs[g % 2]])
            else:
                P.copy("dve", accw[:, g, :, :], psum[bank][:, 0:260].rearrange("p (a b) -> p a b", a=4), reads=[Bps[bank]], writes=[Baccw[g]])

        for i in range(NT):
            qs = slice(i * 128, (i + 1) * 128)
            P.dma(yo[:], src_d[i * 128:(i + 1) * 128, :], writes=[Byo], q="pool")
            for g in range(4):
                build_qpad(i, g)
            for gp in range(2):
                for g in (2 * gp, 2 * gp + 1):
                    extra = [(shb[:, i, :], gsm[:, 4 * g:4 * g + 4, :].rearrange("p a b -> p (a b)"), [Bk1, Bgsm])]
                    pb = scores(i, g, kcmpT, Bkc, 127, slice(0, 127), extra)
                    P.actv(PTf[0:127, g % 2, :], psum[pb][0:127, :], AF.Exp, reads=[Bps[pb]], writes=[BPTf[g % 2]])
                if gp == 0:
                    ck("N3")
                    branch_tiles(i, 0, 2)
                    branch_tiles(i, 1, 2)
                    ck("N4")
                else:
                    branch_tiles(i, 2, 2)
                    branch_tiles(i, 3, 2)
                for g in (2 * gp, 2 * gp + 1):
                    for b in range(4):
                        P.mm(psum[4][:, b * 97:(b + 1) * 97], PTf[0:127, g % 2, b * 128:(b + 1) * 128], vcmp[0:127, g, :],
                             start=(b == 0), stop=True, reads=[BPTf[g % 2], Bvc], writes=[Bps[4]], skip_group_check=True)
                    P.copy("dve", accc[:, g, :, :], psum[4][:, 0:388].rearrange("p (a b) -> p a b", a=4), reads=[Bps[4]], writes=[Baccc])
            P.ts("dve", rec[:, 0, :], accc[:, :, :, 64].rearrange("p g b -> p (g b)"), TINY, None, ALU.add, reads=[Baccc], writes=[Brec])
            P.op("dve", lambda e: e.reciprocal(out=rec[:, 0, :], in_=rec[:, 0, :]), reads=[Brec], writes=[Brec])
            if i >= 4:
                n_before = len(P.segs[-1])
                P.tt("dve", impt, accc[:, :, :, 65:97], bc(rec[:, 0, :].rearrange("p (g b) -> p g b", g=4).unsqueeze(3), [128, 4, 4, 32]),
                     ALU.mult, reads=[Baccc, Brec], writes=[Btmpo])
                P.tt("dve", imp[:], impt[:, :, 0, :], impt[:, :, 1, :], ALU.add, reads=[Btmpo], writes=[Bimp])
                P.tt("dve", imp[:], imp[:], impt[:, :, 2, :], ALU.add, reads=[Bimp, Btmpo], writes=[Bimp])
                P.tt("dve", imp[:], imp[:], impt[:, :, 3, :], ALU.add, reads=[Bimp, Btmpo], writes=[Bimp])
                P.tt("dve", imp[:], imp[:], bc(cmask[:, i, :].unsqueeze(1), [128, 4, 32]), ALU.mult, reads=[Bimp, Bk1], writes=[Bimp])
                P.tt("dve", imp[:], imp[:], bc(fbias[:, i, :].unsqueeze(1), [128, 4, 32]), ALU.add, reads=[Bimp, Bk1], writes=[Bimp])
                for g in range(4):
                    P.op("dve", lambda e, g=g: e.max(out=mx8[:, g, :], in_=imp[:, g, :]), reads=[Bimp], writes=[Bimp])
                    P.ts("dve", selb[:, g, :], imp[:, g, :], mx8[:, g, 7:8], -BIG, ALU.is_lt, ALU.mult, reads=[Bimp], writes=[Bselb])
                pst = psum[7][:].bitcast(BF16)
                for g in range(4):
                    P.tr(pst[0:32, g * 128:(g + 1) * 128], selb[:, g, :], identb[:], reads=[Bselb, Bcst], writes=[Bps[7]])
                for g in range(4):
                    P.copy("act" if g % 2 else "dve", selbT[0:32, g, :], pst[0:32, g * 128:(g + 1) * 128], reads=[Bps[7]], writes=[BselbT[g]])
                for o_ in P.segs[-1][n_before:]:
                    if o_.eng != "pe":
                        o_.cost *= 2.5
            ck("N5")
            P.tt("dve", rec[:, 0, :], rec[:, 0, :], gts[:, i, 0:16], ALU.mult, reads=[Brec, Bgts], writes=[Brec])
            for g in range(4):
                branch_tiles(i, g, 1)
                gi = g % 2
                P.ts("dve", recsw[:, 0, :], accs[gi][:, :, 64], TINY, None, ALU.add, reads=[Baccs[gi]], writes=[Brecsw])
                P.ts("dve", recsw[:, 1, :], accw[:, g, :, 64], TINY, None, ALU.add, reads=[Baccw[g]], writes=[Brecsw])
                P.op("dve", lambda e: e.reciprocal(out=recsw[:], in_=recsw[:]), reads=[Brecsw], writes=[Brecsw])
                P.tt("dve", recsw[:], recsw[:], gts[:, i, 16:48].rearrange("p (r h) -> p r h", r=2)[:, :, 4 * g:4 * g + 4], ALU.mult,
                     reads=[Brecsw, Bgts], writes=[Brecsw])
                P.tt("pool", tmpo[:, 0, :, :], accc[:, g, :, 0:64], bc(rec[:, 0, 4 * g:4 * g + 4].unsqueeze(2), [128, 4, 64]), ALU.mult,
                     reads=[Baccc, Brec], writes=[Btmpo])
                for r in range(2):
                    src_, Bsrc_ = (accs[gi][:, :, 0:64], Baccs[gi]) if r == 0 else (accw[:, g, :, 0:64], Baccw[g])
                    P.tt("pool", tmpo[:, 1, :, :], src_, bc(recsw[:, r, :].unsqueeze(2), [128, 4, 64]), ALU.mult,
                         reads=[Bsrc_, Brecsw], writes=[Btmpo])
                    P.tt("pool", tmpo[:, 0, :, :], tmpo[:, 0, :, :], tmpo[:, 1, :, :], ALU.add, reads=[Btmpo], writes=[Btmpo])
                P.tt("pool", og[:, g * 256:(g + 1) * 256], tmpo[:, 0, :, :].rearrange("p h d -> p (h d)"), zsn[:, i, g * 256:(g + 1) * 256],
                     ALU.mult, reads=[Btmpo, Bzsn], writes=[Bog])
            ck("N6")
            pst = psum[7][:].bitcast(BF16)
            for c in range(8):
                P.tr(pst[:, c * 128:(c + 1) * 128], og[:, c * 128:(c + 1) * 128], identb[:], reads=[Bog, Bcst], writes=[Bps[7]])
            P.copy("act", ogT[:].rearrange("p a b -> p (a b)"), pst[:, 0:1024], reads=[Bps[7]], writes=[BogT])
            for hh in range(2):
                pb = 7
                for c in range(8):
                    P.mm(psum[pb][:, :], ogT[:, c, :], wo[:, c, hh * 512:(hh + 1) * 512], start=(c == 0), stop=(c == 7),
                         reads=[BogT, Bwo], writes=[Bps[pb]])
                tview = tmpo[:].rearrange("p a b c -> p (a b c)")
                P.tt("dve", tview, psum[pb][:, :], gate_bc[:, hh * 512:(hh + 1) * 512], ALU.mult,
                     reads=[Bps[pb], Bmod], writes=[Btmpo])
                P.tt("pool", yo[:, hh * 512:(hh + 1) * 512], yo[:, hh * 512:(hh + 1) * 512], tview, ALU.add, reads=[Btmpo, Byo], writes=[Byo])
            P.actv(junk, yo[:], AF.Square, reads=[Byo], writes=[Btmpo, Bfst], accum_out=fst[:, i, 0:1])
            P.ts("dve", fst[:, i, 1:2], fst[:, i, 0:1], 1.0 / D, EPS, ALU.mult, ALU.add, reads=[Bfst], writes=[Bfst])
            P.actv(fst[:, i, 1:2], fst[:, i, 1:2], AF.Ln, reads=[Bfst], writes=[Bfst])
            P.actv(fst[:, i, 2:3], fst[:, i, 1:2], AF.Exp, reads=[Bfst], writes=[Bfst], scale=-0.5)
            P.stt("dve", yo[:], yo[:], fst[:, i, 2:3], fgb[:], ALU.mult, ALU.mult, reads=[Byo, Bfst, Bk1], writes=[Byo])
            o = P.dma(dst_d[i * 128:(i + 1) * 128, :], yo[:], reads=[Byo], q="sp")
            final_ops.append(o)
            ck("N7")
        A.top = top0
        A.release(mL)

    try:
        if mode == "l0":
            layer_gdn(x_in, out_d)
        elif mode == "l1":
            layer_nsa(x_in, out_d)
        else:
            layer_gdn(x_in, x1_d)
            layer_nsa(x1_d, out_d)
    except _Stop:
        P.barrier()
        final_ops.append(P.dma(out_d[0:128, 0:16], cst[:, 0, 0:16], reads=[Bcst]))
    P.emit(final_dma_ops=final_ops + tap_ops)
    if os.environ.get("KVERBOSE"):
        print("arena peak bytes", getattr(A, "peak", 0), "of", nc.sbuf_top - A.base, "est_us", getattr(P, "est_time", 0.0))
    es.close()
    return nc


def _rel_bucket(d):
    n = np.maximum(d, 0)
    nf = np.maximum(n, 1).astype(np.float32)
    large = 16 + (np.log(nf / np.float32(16)) / np.float32(math.log(128 / 16)) * np.float32(16)).astype(np.int32)
    large = np.minimum(large, 31)
    return np.where(n < 16, n, large)


def make_nsa_consts():
    c = {}
    d = np.arange(384) - 128
    bk = _rel_bucket(d)
    oh = np.zeros((33, 384), np.float32)
    for j in range(384):
        if d[j] >= 0:
            oh[bk[j], j] += 1.0
            oh[31, j] -= 1.0
        else:
            oh[32, j] = -BIG
    c["nsa_oh"] = oh
    sh = np.zeros((128, NT, 127), np.float32)
    for i in range(NT):
        for blk in range(127):
            rp = blk - 8 * i
            if -8 <= rp <= 6:
                sh[rp + 8, i, blk] = 1.0
            elif rp >= 7:
                sh[15, i, blk] = 1.0
    c["nsa_sh"] = sh
    em = np.zeros((128, NT, 128), np.float32)
    for j in range(NT):
        for key in range(128):
            em[2 * j + key // 64, j, key] = 1.0
    c["nsa_em"] = em
    p = np.arange(128)
    c["nsa_wm"] = np.where(p[:, None] > p[None, :], 0.0, -BIG).astype(np.float32)
    cm = np.zeros((128, NT, 32), np.float32)
    fb = np.zeros((128, NT, 32), np.float32)
    for i in range(NT):
        q = 128 * i + p
        cur = q // 64
        s = np.arange(32)
        forced = (s[None, :] == 0) | (s[None, :] == cur[:, None]) | (s[None, :] == cur[:, None] - 1)
        causal = s[None, :] * 64 <= q[:, None]
        cm[:, i, :] = (causal & ~forced) * 1.0
        fb[:, i, :] = np.where(forced, 1e30, np.where(causal, 0.0, -1e30))
    c["nsa_cmask"] = cm
    c["nsa_fbias"] = fb
    cs = np.arange(127)[:, None] * 16
    ss = np.arange(32)[None, :] * 64
    ov = np.clip(np.minimum(cs + 32, ss + 64) - np.maximum(cs, ss), 0, None).astype(np.float32) / 32.0
    c["nsa_ovl"] = np.ascontiguousarray(ov)
    return c


def _permute_nsa_w(w):
    cols = []
    for g in range(4):
        for hd in (4 * g, 4 * g + 2, 4 * g + 1, 4 * g + 3):
            cols += list(range(hd * 64, hd * 64 + 64))
    kv0 = 1024

    def kvcols(i, g):
        return list(range(kv0 + i * 256 + g * 64, kv0 + i * 256 + g * 64 + 64))
    for i in (2, 4):
        for g in range(4):
            cols += kvcols(i, g) + kvcols(i, g)
    for i in (0, 1):
        for g in range(4):
            cols += kvcols(i, g)
    for i in (3, 5):
        for g in range(4):
            cols += kvcols(i, g)
    off = 1024 + 6 * 256
    cols += list(range(off + 48, off + 48 + 1024))
    for br in range(3):
        for hd in range(16):
            cols.append(off + hd * 3 + br)
    return np.ascontiguousarray(w[:, np.asarray(cols)])


_CACHE = {}


def _prep_common(inputs):
    f = lambda a: np.ascontiguousarray(np.asarray(a, dtype=np.float32))
    com = {
        "ada_w": f(inputs["ada_w"]),
        "ada_b": f(np.asarray(inputs["ada_b"]).reshape(2, 24, 128).transpose(0, 2, 1)),
        "ada_b_row": f(inputs["ada_b"]),
        "norm_g": f(np.asarray(inputs["norm_g"]).reshape(2, 8, 128).transpose(0, 2, 1)),
        "gdn_w_in": f(inputs["gdn_w_in"][0]),
        "gdn_cw": f(np.asarray(inputs["gdn_conv_w"][0]).reshape(4, 32, 128).transpose(2, 1, 0)),
        "gdn_alog": f(np.asarray(inputs["gdn_a_log"]).reshape(1, 16)),
        "gdn_dtb": f(np.asarray(inputs["gdn_dt_bias"]).reshape(1, 16)),
        "gdn_normw": f(np.asarray(inputs["gdn_norm_w"]).reshape(1, 128)),
        "gdn_w_out": f(inputs["gdn_w_out"][0]),
        "final_g": f(np.asarray(inputs["final_g"]).reshape(1, D)),
        "cst": make_consts(),
        "nsa_w": _permute_nsa_w(f(inputs["nsa_w_in"][0])),
        "nsa_w1": f(inputs["nsa_cmp_w1"][0]),
        "nsa_w2": f(inputs["nsa_cmp_w2"][0]),
        "nsa_pos": f(inputs["nsa_cmp_pos"][0]),
        "nsa_wo": f(inputs["nsa_w_out"][0]),
        "rel_bias": f(inputs["rel_bias"]),
    }
    com.update(make_nsa_consts())
    return com


def kernel(**inputs):
    x = np.asarray(inputs["x"], dtype=np.float32)
    c = np.asarray(inputs["c"], dtype=np.float32)
    com = _prep_common(inputs)
    if "all" not in _CACHE:
        _CACHE["all"] = build("all")
    nc = _CACHE["all"]
    in_maps = []
    for b in range(8):
        m = dict(com)
        m["x"] = np.ascontiguousarray(x[b])
        m["cvec"] = np.ascontiguousarray(c[b].reshape(8, 128).T)
        in_maps.append(m)
    res = run_bass_kernel_spmd(nc, in_maps, core_ids=list(range(8)))
    return np.stack([res.results[b]["out"] for b in range(8)], axis=0)
```

```python
import math
from contextlib import ExitStack

import numpy as np
import concourse.bass as bass
import concourse.mybir as mybir
from concourse.ap import AP
from concourse.bass_utils import run_bass_kernel_spmd

F32 = mybir.dt.float32
F32R = mybir.dt.float32r
BF16 = mybir.dt.bfloat16
AF = mybir.ActivationFunctionType
ALU = mybir.AluOpType

D = 1024
T = 2048
NT = 16
EPS = 1e-6
GDN_IN = 6176
NSA_IN = 3632
BIG = 30000.0


class Buf:
    __slots__ = ("name", "w", "r", "excl")

    def __init__(self, name="", excl=False):
        self.name = name
        self.w = {}
        self.r = []
        self.excl = excl


class Op:
    __slots__ = ("eng", "fn", "deps", "signal", "is_dma", "dma_id", "seq", "semval", "cost", "prio", "t0", "t1",
                 "nsucc", "succs", "npend", "ready", "tab")

    def __init__(self, eng, fn, is_dma=False, cost=0.3):
        self.eng = eng
        self.fn = fn
        self.deps = {}
        self.signal = False
        self.is_dma = is_dma
        self.dma_id = None
        self.seq = 0
        self.semval = None
        self.cost = cost
        self.prio = 0.0
        self.t0 = 0.0
        self.t1 = 0.0
        self.succs = []
        self.npend = 0
        self.ready = 0.0
        self.tab = None


ENGS = ("pe", "act", "dve", "pool", "sp")
CENGS = ("pe", "act", "dve", "pool")
ND = 32
SYNC_LAT = 0.30
DMA_LAT = 2.0


class Prog:
    def __init__(self, nc, schedule=True):
        self.nc = nc
        self.segs = [[]]
        self.nops = 0
        self.schedule = schedule

    def _add_dep(self, op, dep):
        if dep is op:
            return
        order_only = (dep.eng == "pe" and op.eng == "pe" and not dep.is_dma and not op.is_dma)
        if dep.is_dma and op.is_dma and False:
            return
        prev = op.deps.get(dep)
        if prev is None or (prev is False and not order_only):
            op.deps[dep] = not order_only

    def op(self, eng, fn, reads=(), writes=(), is_dma=False, cost=0.3):
        o = Op(eng, fn, is_dma, cost)
        o.seq = self.nops
        self.nops += 1
        if any(b.excl for b in reads):
            writes = list(writes) + [b for b in reads if b.excl]
            reads = [b for b in reads if not b.excl]
        for b in reads:
            for d in b.w.values():
                self._add_dep(o, d)
        for b in writes:
            for d in b.w.values():
                self._add_dep(o, d)
            for d in b.r:
                self._add_dep(o, d)
        key = ("dma", o.seq) if is_dma else eng
        for b in reads:
            b.r.append(o)
        for b in writes:
            b.w = {key: o}
            b.r = []
        self.segs[-1].append(o)
        return o

    def barrier(self):
        if self.segs[-1]:
            self.segs.append([])

    def _schedule_segment(self, ops):
        import heapq
        inseg = set(ops)
        for o in ops:
            o.succs = []
        for o in ops:
            o.deps = {d: s for d, s in o.deps.items() if d in inseg}
            o.npend = len(o.deps)
            for d in o.deps:
                d.succs.append(o)
        if not self.schedule:
            return {e: [o for o in ops if o.eng == e] for e in ENGS}
        for o in reversed(ops):
            m = 0.0
            for s in o.succs:
                if s.prio > m:
                    m = s.prio
            o.prio = m + (DMA_LAT if o.is_dma else o.cost)
            o.ready = 0.0
        free = {e: 0.0 for e in ENGS}
        hA = {e: [] for e in ENGS}
        hB = {e: {} for e in ENGS}
        cur_tab = [None]
        TAB_PEN, TAB_COST = 6.0, 1.3
        for o in ops:
            if o.npend == 0:
                heapq.heappush(hA[o.eng], (0.0, -o.prio, o.seq, o))
        out = {e: [] for e in ENGS}
        remaining = len(ops)
        while remaining:
            best = None
            for e in ENGS:
                a, b = hA[e], hB[e]
                while a and a[0][0] <= free[e]:
                    _, np_, sq, o = heapq.heappop(a)
                    heapq.heappush(b.setdefault(o.tab, []), (np_, sq, o))
                bt = None
                for tb_, hp in b.items():
                    if not hp:
                        continue
                    sc = -hp[0][0] - (TAB_PEN if (e == "act" and tb_ is not None and tb_ != cur_tab[0]) else 0.0)
                    if bt is None or sc > bt[0]:
                        bt = (sc, tb_)
                if bt is not None:
                    cand = (free[e], -bt[0], e, bt[1], True)
                elif a:
                    cand = (a[0][0], a[0][1], e, None, False)
                else:
                    continue
                if best is None or cand[:2] < best[:2]:
                    best = cand
            st, _, e, tb_, fromb = best
            if fromb:
                _, _, o = heapq.heappop(hB[e][tb_])
            else:
                _, _, _, o = heapq.heappop(hA[e])
            if e == "act" and o.tab is not None and o.tab != cur_tab[0]:
                st += TAB_COST
                cur_tab[0] = o.tab
            o.t0 = st
            if o.is_dma:
                free[e] = st + 0.06
                o.t1 = st + DMA_LAT + o.cost
            else:
                o.t1 = st + o.cost
                free[e] = o.t1
            out[e].append(o)
            remaining -= 1
            for s_ in o.succs:
                t = (o.t1 + SYNC_LAT) if s_.deps[o] else o.t0 + 0.01
                if t > s_.ready:
                    s_.ready = t
                s_.npend -= 1
                if s_.npend == 0:
                    heapq.heappush(hA[s_.eng], (s_.ready, -s_.prio, s_.seq, s_))
        self.est_time = getattr(self, "est_time", 0.0) + max(o.t1 for o in ops)
        return out

    def emit(self, final_dma_ops=()):
        nc = self.nc
        segs = [sg for sg in self.segs if sg]
        order = {e: [] for e in ENGS}
        dma_order = []
        for sg in segs:
            sch = self._schedule_segment(sg)
            seg_dmas = [o for o in sg if o.is_dma]
            seg_dmas.sort(key=lambda o: (o.t0, o.seq))
            dma_order += seg_dmas
            lasts = {}
            for e in CENGS:
                for o in reversed(sch[e]):
                    if not o.is_dma:
                        lasts[e] = o
                        break
            for e in ENGS:
                order[e] += [("op", o) for o in sch[e]]
                order[e].append(("bar", (lasts, seg_dmas)))
            for o in lasts.values():
                o.signal = True
        for j, o in enumerate(dma_order):
            o.dma_id = j
        for e in ENGS:
            for kind, o in order[e]:
                if kind == "op":
                    for d, needs in o.deps.items():
                        if needs and not d.is_dma:
                            d.signal = True
        with ExitStack() as es:
            sems = {e: es.enter_context(nc.semaphore("s_" + e)) for e in CENGS}
            dsems = [es.enter_context(nc.semaphore("d%d" % i)) for i in range(ND)]
            for e in ENGS:
                c = 0
                for kind, o in order[e]:
                    if kind != "op" or o.is_dma:
                        continue
                    if o.signal:
                        c += 1
                    o.semval = c
            block = es.enter_context(nc.Block())
            deco = {"pe": block.tensor, "act": block.scalar, "dve": block.vector,
                    "pool": block.gpsimd, "sp": block.sync}

            def run_engine(ename):
                def body(eng):
                    waited = {}

                    def wait(sem, val):
                        if waited.get(sem.name, -1) >= val:
                            return
                        waited[sem.name] = val
                        eng.wait_ge(sem, val)

                    def wait_dma(d):
                        j = d.dma_id
                        wait(dsems[j % ND], 16 * (j // ND + 1))

                    for kind, o in order[ename]:
                        if kind == "bar":
                            lasts, seg_dmas = o
                            for e2, lo in lasts.items():
                                wait(sems[e2], lo.semval)
                            for d in seg_dmas:
                                wait_dma(d)
                            continue
                        for d, needs in o.deps.items():
                            if not needs:
                                continue
                            if d.is_dma:
                                wait_dma(d)
                            else:
                                wait(sems[d.eng], d.semval)
                        if o.is_dma:
                            j = o.dma_id
                            if j >= ND:
                                wait(dsems[j % ND], 16 * (j // ND))
                            o.fn(eng).then_inc(dsems[j % ND], 16)
                        else:
                            ins = o.fn(eng)
                            if o.signal:
                                ins.then_inc(sems[ename], 1)
                    if ename == "sp":
                        for o in final_dma_ops:
                            wait_dma(o)
                return body

            for ename in ENGS:
                deco[ename](run_engine(ename))

    @staticmethod
    def _n(ap):
        try:
            return ap.free_size()
        except Exception:
            return 128

    def mm(self, out, lhsT, rhs, start=True, stop=True, reads=(), writes=(), **kw):
        n = self._n(out)
        f32 = (lhsT.dtype == F32)
        cost = max(n, 48) * (4.0 if f32 else 1.0) / 1900.0 + 0.02
        return self.op("pe", lambda e: e.matmul(out, lhsT=lhsT, rhs=rhs, start=start, stop=stop, **kw), reads, writes, cost=cost)

    def tr(self, out, in_, ident, reads=(), writes=()):
        cost = 128 * (2.0 if in_.dtype == F32 else 1.0) / 1900.0 + 0.05
        return self.op("pe", lambda e: e.transpose(out=out, in_=in_, identity=ident), reads, writes, cost=cost)

    _TABS = {AF.Exp: "exp", AF.Silu: "silu", AF.Sqrt: "sqrt", AF.Sigmoid: "sigmoid", AF.Ln: "ln", AF.Tanh: "exp"}

    def actv(self, out, in_, func, reads=(), writes=(), **kw):
        o = self.op("act", lambda e: e.activation(out=out, in_=in_, func=func, **kw), reads, writes, cost=0.28 + self._n(out) * 0.00085)
        o.tab = self._TABS.get(func)
        return o

    def _vcost(self, eng, out):
        n = self._n(out)
        if eng == "pool":
            return 0.5 + n * 0.0021
        if eng == "act":
            return 0.28 + n * 0.00085
        return 0.2 + n * 0.00105

    def copy(self, eng, out, in_, reads=(), writes=()):
        if eng == "act":
            return self.op("act", lambda e: e.activation(out=out, in_=in_, func=AF.Copy), reads, writes, cost=self._vcost(eng, out))
        return self.op(eng, lambda e: e.tensor_copy(out=out, in_=in_), reads, writes, cost=self._vcost(eng, out))

    def tt(self, eng, out, in0, in1, op, reads=(), writes=()):
        return self.op(eng, lambda e: e.tensor_tensor(out=out, in0=in0, in1=in1, op=op), reads, writes, cost=self._vcost(eng, out))

    def ts(self, eng, out, in0, s1, s2, op0, op1=None, reads=(), writes=()):
        if eng == "act":
            assert op0 == ALU.mult and op1 is None
            return self.op("act", lambda e: e.activation(out=out, in_=in0, func=AF.Copy, scale=s1), reads, writes, cost=self._vcost(eng, out))
        if op1 is None:
            return self.op(eng, lambda e: e.tensor_scalar(out=out, in0=in0, scalar1=s1, scalar2=None, op0=op0), reads, writes,
                           cost=self._vcost(eng, out))
        return self.op(eng, lambda e: e.tensor_scalar(out=out, in0=in0, scalar1=s1, scalar2=s2, op0=op0, op1=op1), reads, writes,
                       cost=self._vcost(eng, out))

    def stt(self, eng, out, in0, scalar, in1, op0, op1, reads=(), writes=()):
        return self.op(eng, lambda e: e.scalar_tensor_tensor(out=out, in0=in0, scalar=scalar, in1=in1, op0=op0, op1=op1), reads, writes,
                       cost=self._vcost(eng, out))

    def dma(self, out, in_, reads=(), writes=(), q="sp", **kw):
        try:
            nbytes = out.size() * mybir.dt.size(out.dtype)
        except Exception:
            nbytes = 1 << 16
        return self.op(q, lambda e: e.dma_start(out=out, in_=in_, **kw), reads, writes, is_dma=True, cost=nbytes / 100e3)


class Arena:
    def __init__(self, nc):
        self.nc = nc
        self.base = (nc.sbuf_base + 31) // 32 * 32
        self.top = nc.sbuf_top
        self.off = self.base
        self.n = 0
        self.prog = None

    def alloc(self, shape, dtype, name=None):
        sz = int(np.prod(shape[1:])) * mybir.dt.size(dtype)
        sz = (sz + 31) // 32 * 32
        if self.off + sz > self.top:
            raise RuntimeError("SBUF arena overflow at %s: need %d, have %d" % (name, sz, self.top - self.off))
        self.n += 1
        self.peak = max(getattr(self, "peak", 0), self.off + sz - self.base + (self.nc.sbuf_top - self.top))
        t = self.nc.alloc_sbuf_tensor_at("%s_%d" % (name or "t", self.n), list(shape), dtype, offset=self.off)
        self.off += sz
        return t

    def alloc_top(self, shape, dtype, name=None):
        sz = int(np.prod(shape[1:])) * mybir.dt.size(dtype)
        sz = (sz + 31) // 32 * 32
        if self.top - sz < self.off:
            raise RuntimeError("SBUF arena overflow (top) at %s: need %d, have %d" % (name, sz, self.top - self.off))
        self.n += 1
        self.top -= sz
        return self.nc.alloc_sbuf_tensor_at("%s_%d" % (name or "t", self.n), list(shape), dtype, offset=self.top)

    def mark(self):
        return self.off

    def release(self, m):
        self.off = m
        if self.prog is not None:
            self.prog.barrier()


def bc(ap, shape):
    return ap.broadcast_to(list(shape))


CST_NAMES = ["ident", "ones", "u64", "onesbd", "ones_a", "ones_b", "mask_a", "mask_q"]


def make_consts():
    p = np.arange(128)
    same = (p[:, None] // 64) == (p[None, :] // 64)
    c = {}
    c["ident"] = np.eye(128)
    c["ones"] = np.ones((128, 128))
    c["u64"] = ((p[:, None] <= p[None, :]) & same) * 1.0
    c["onesbd"] = same * 1.0
    c["ones_a"] = np.repeat((p < 64)[:, None] * 1.0, 128, 1)
    c["ones_b"] = np.repeat((p >= 64)[:, None] * 1.0, 128, 1)
    c["mask_a"] = np.where((p[None, :] < p[:, None]) & same, 0.0, BIG)
    c["mask_q"] = np.where((p[None, :] >= p[:, None]) & same, 0.0, -BIG)
    return np.stack([c[n] for n in CST_NAMES], axis=1).astype(np.float32)


class _Stop(Exception):
    pass


def build(mode="all", dbg=None, neumann_dt=F32, stop=None, taps=()):
    nc = bass.Bass("TRN2", target_bir_lowering=False)
    tap_ops = []

    def tap(name, ap, reads):
        if name not in taps:
            return
        dt_ = nc.dram_tensor(name, list(ap.shape), ap.dtype, kind="ExternalOutput").ap()
        tap_ops.append(P.dma(dt_, ap, reads=reads, q="sp"))

    def ck(name):
        if stop == name:
            raise _Stop()

    dram = {}

    def din(name, shape, dt=F32):
        dram[name] = nc.dram_tensor(name, list(shape), dt, kind="ExternalInput").ap()
        return dram[name]

    x_in = din("x", [T, D])
    cvec = din("cvec", [128, 8])
    ada_w = din("ada_w", [2, D, 3 * D])
    ada_b = din("ada_b", [2, 128, 24])
    ada_b_row = din("ada_b_row", [2, 3 * D])
    norm_g = din("norm_g", [2, 128, 8])
    gdn_w_in = din("gdn_w_in", [D, GDN_IN])
    gdn_cw = din("gdn_cw", [128, 32, 4])
    gdn_alog = din("gdn_alog", [1, 16])
    gdn_dtb = din("gdn_dtb", [1, 16])
    gdn_normw = din("gdn_normw", [1, 128])
    gdn_w_out = din("gdn_w_out", [2048, D])
    final_g = din("final_g", [1, D])
    nsa_w = din("nsa_w", [D, 4144])
    nsa_w1 = din("nsa_w1", [2, 2048, 64])
    nsa_w2 = din("nsa_w2", [2, 64, 64])
    nsa_pos = din("nsa_pos", [2, 32, 64])
    nsa_wo = din("nsa_wo", [D, D])
    rel_bias_d = din("rel_bias", [32, 16])
    nsa_oh = din("nsa_oh", [33, 384])
    nsa_sh = din("nsa_sh", [128, NT, 127])
    nsa_em = din("nsa_em", [128, NT, 128])
    nsa_wm = din("nsa_wm", [128, 128])
    nsa_cmask = din("nsa_cmask", [128, NT, 32])
    nsa_fbias = din("nsa_fbias", [128, NT, 32])
    nsa_ovl = din("nsa_ovl", [127, 32])
    x1_d = nc.dram_tensor("x1_scratch", [T, D], F32).ap()
    og_d = nc.dram_tensor("og_scratch", [T, 2048], BF16).ap()
    zt_d = nc.dram_tensor("zt_scratch", [16, 128, 400], F32).ap()
    cst_d = din("cst", [128, len(CST_NAMES), 128])
    out_d = nc.dram_tensor("out", [T, D], F32, kind="ExternalOutput").ap()
    dbg_d = None
    if dbg is not None:
        dbg_d = nc.dram_tensor("dbg", list(dbg), F32, kind="ExternalOutput").ap()

    import os
    P = Prog(nc, schedule=(os.environ.get("KSCHED", "1") == "1"))
    A = Arena(nc)
    A.prog = P
    es = ExitStack()
    psum = [es.enter_context(nc.psum_tensor("ps%d" % i, [128, 512], F32)) for i in range(8)]
    final_ops = []

    cst = A.alloc([128, len(CST_NAMES), 128], F32, "cst")
    Bcst = Buf("cst")
    P.dma(cst[:], cst_d, writes=[Bcst])
    C = {n: cst[:, i, :] for i, n in enumerate(CST_NAMES)}
    identb = A.alloc([128, 128], BF16, "identb")
    onesb = A.alloc([128, 128], BF16, "onesb")
    P.copy("dve", identb[:], C["ident"], reads=[Bcst], writes=[Bcst])
    P.copy("dve", onesb[:], C["ones"], reads=[Bcst], writes=[Bcst])

    cond = A.alloc([128, 8], F32, "cond")
    modsb = A.alloc([128, 2, 24], F32, "mod")
    sc1 = A.alloc([128, 2, 8], F32, "sc1")
    adab = A.alloc([128, 2, 24], F32, "adab")
    normg = A.alloc([128, 2, 8], F32, "normg")
    gate_bc = A.alloc([128, D], F32, "gate_bc")
    Bsm = Buf("small")
    Bmod = Buf("mod")
    P.dma(cond[:], cvec, writes=[Bsm])
    P.dma(adab[:], ada_b.rearrange("l p j -> p l j"), writes=[Bsm])
    P.dma(normg[:], norm_g.rearrange("l p j -> p l j"), writes=[Bsm])
    P.actv(cond[:], cond[:], AF.Silu, reads=[Bsm], writes=[Bsm])
    Bps = [Buf("ps%d" % i, excl=True) for i in range(8)]
    Bmrow = Buf("modrow")

    def stage_a(wsa, Bwsa, modrow, adabrow, layers, nblk0=6):
        P.dma(adabrow[:], ada_b_row.rearrange("(o l) n -> o l n", o=1), writes=[Bmrow])
        cnt = 0
        for l in layers:
            nb_ = nblk0 if l == 0 else 6
            for cb_ in range(nb_):
                i = cnt % 2
                cnt += 1
                for kh in range(2):
                    P.dma(wsa[i][:, kh * 4:(kh + 1) * 4, :],
                          ada_w[l, kh * 512:(kh + 1) * 512, cb_ * 512:(cb_ + 1) * 512].rearrange("(kc p) n -> p kc n", p=128),
                          writes=[Bwsa[i]], q=("sp" if kh == 0 else "pool"))
                pb = i
                for kc in range(8):
                    P.mm(psum[pb][0:1, :], cond[:, kc:kc + 1], wsa[i][:, kc, :], start=(kc == 0), stop=(kc == 7),
                         reads=[Bwsa[i], Bsm], writes=[Bps[pb]])
                P.tt("dve", modrow[:, cb_ * 512:(cb_ + 1) * 512], psum[pb][0:1, :], adabrow[:, l, cb_ * 512:(cb_ + 1) * 512], ALU.add,
                     reads=[Bps[pb], Bmrow], writes=[Bmrow])
            nj = nb_ * 4
            for j in range(nj):
                P.mm(psum[2][:, j:j + 1], modrow[0:1, j * 128:(j + 1) * 128], C["ones"][0:1, 0:1], reads=[Bmrow, Bcst], writes=[Bps[2]])
            P.copy("dve", modsb[:, l, 0:nj], psum[2][:, 0:nj], reads=[Bps[2]], writes=[Bmod])
            P.stt("dve", sc1[:, l, :], modsb[:, l, 8:16], 1.0, normg[:, l, :], ALU.add, ALU.mult, reads=[Bmod, Bsm], writes=[Bmod])

    def stage_a_col(l, wsc, Bwsc, fcs=range(24), with_sc1=True):
        for fc in fcs:
            i = fc % 2
            for kh in range(2):
                P.dma(wsc[i][:, kh * 4:(kh + 1) * 4, :],
                      ada_w[l, kh * 512:(kh + 1) * 512, fc * 128:(fc + 1) * 128].rearrange("(kc p) n -> p kc n", p=128),
                      writes=[Bwsc[i]], q="sp")
            for kc in range(8):
                P.mm(psum[0][:, 0:1], wsc[i][:, kc, :], cond[:, kc:kc + 1], start=(kc == 0), stop=(kc == 7),
                     reads=[Bwsc[i], Bsm], writes=[Bps[0]])
            P.tt("dve", modsb[:, l, fc:fc + 1], psum[0][:, 0:1], adab[:, l, fc:fc + 1], ALU.add, reads=[Bps[0], Bsm], writes=[Bmod])
        if with_sc1:
            P.stt("dve", sc1[:, l, :], modsb[:, l, 8:16], 1.0, normg[:, l, :], ALU.add, ALU.mult, reads=[Bmod, Bsm], writes=[Bmod])
            stage_a_done[l] = True

    stage_a_done = [False, False]
    stage_a_mark = [None]

    def run_stage_a(layers):
        layers = [l_ for l_ in layers if not stage_a_done[l_]]
        stage_a_mark[0] = None
        if not layers:
            return
        for l_ in layers:
            stage_a_done[l_] = True
        stage_a_mark[0] = A.mark()
        wsa = [A.alloc([128, 8, 512], F32, "wsa%d" % i) for i in range(2)]
        modrow = A.alloc([1, 3 * D], F32, "modrow")
        adabrow = A.alloc([1, 2, 3 * D], F32, "adabrow")
        stage_a(wsa, [Buf(), Buf()], modrow, adabrow, layers, nblk0=(4 if mode == "all" else 6))

    def make_gate_bc(l):
        m = A.mark()
        tmp = A.alloc([128, 8, 128], F32, "gtmp")
        Bt = Buf()
        P.tt("pool", tmp[:], bc(C["ident"].unsqueeze(1), [128, 8, 128]),
             bc(modsb[:, l, 16:24].unsqueeze(2), [128, 8, 128]), ALU.mult, reads=[Bcst, Bmod], writes=[Bt])
        for hh in range(2):
            P.mm(psum[hh][:, :], C["ones"], tmp[:, hh * 4:(hh + 1) * 4, :].rearrange("p a b -> p (a b)"),
                 reads=[Bt, Bcst], writes=[Bps[hh]])
            P.copy("act", gate_bc[:, hh * 512:(hh + 1) * 512], psum[hh][:, :], reads=[Bps[hh]], writes=[Bmod])
        A.release(m)

    def make_hT(src_d, l, hT, BhT):
        m = A.mark()
        NX = 3
        xt = [A.alloc([128, D], F32, "xt%d" % i) for i in range(NX)]
        Bxt = [Buf() for _ in range(NX)]
        xs = [A.alloc([128, D], BF16, "xs%d" % i) for i in range(NX)]
        Bxs = [Buf() for _ in range(NX)]
        junk = A.alloc([128, D], BF16, "junk")
        Bj = Buf()
        st = A.alloc([128, NT, 3], F32, "st")
        Bst = [Buf() for _ in range(NT)]
        tmp = [A.alloc([128, 8, 128], F32, "htmp%d" % i) for i in range(NX)]
        Btmp = [Buf() for _ in range(NX)]
        for t in range(NT):
            i = t % NX
            P.dma(xt[i][:], src_d[t * 128:(t + 1) * 128, :], writes=[Bxt[i]], q=("sp" if t % 2 == 0 else "pool"))
            P.actv(junk[:], xt[i][:], AF.Square, reads=[Bxt[i]], writes=[Bj, Bst[t]], accum_out=st[:, t, 0:1])
            P.actv(st[:, t, 1:2], st[:, t, 0:1], AF.Sqrt, reads=[Bst[t]], writes=[Bst[t]], scale=1.0 / D, bias=EPS)
            P.op("dve", lambda e, t=t: e.reciprocal(out=st[:, t, 2:3], in_=st[:, t, 1:2]), reads=[Bst[t]], writes=[Bst[t]])
            P.actv(xs[i][:], xt[i][:], AF.Copy, reads=[Bxt[i], Bst[t]], writes=[Bxs[i]], scale=st[:, t, 2:3])
            pb = 2 + t % 3
            pst = psum[pb][:].bitcast(BF16)
            for fc in range(8):
                P.tr(pst[:, fc * 128:(fc + 1) * 128], xs[i][:, fc * 128:(fc + 1) * 128], identb[:],
                     reads=[Bxs[i], Bcst], writes=[Bps[pb]])
            P.tt("dve", tmp[i][:], pst[:, 0:1024].rearrange("p (a b) -> p a b", a=8),
                 bc(sc1[:, l, :].unsqueeze(2), [128, 8, 128]), ALU.mult, reads=[Bps[pb], Bmod], writes=[Btmp[i]])
            P.tt("dve" if t % 3 else "pool", hT[:, :, t * 128:(t + 1) * 128], tmp[i][:],
                 bc(modsb[:, l, 0:8].unsqueeze(2), [128, 8, 128]), ALU.add, reads=[Btmp[i], Bmod], writes=[BhT])
        A.release(m)

    def residual_out(src_d, dst_d, y_fn, final=False):
        pass

    def layer_gdn(src_d, dst_d):
        l = 0
        mL = A.mark()
        hT = A.alloc([128, 8, T], BF16, "hT")
        BhT = Buf("hT")
        run_stage_a([0] if mode == "all" else [0, 1])
        make_hT(src_d, l, hT, BhT)
        A.off = stage_a_mark[0] if stage_a_mark[0] is not None else A.off
        tap("hT", hT[:], [BhT])
        ck("B")
        if mode != "all":
            make_gate_bc(l)
        tap("gate_bc", gate_bc[:], [Bmod])
        ck("B2")
        wo_alias = hT[:].rearrange("p a (b c) -> p (a b) c", c=D)

        NS = 8
        sca = A.alloc([128, NS, NT, 16], F32, "sca")
        Bsc = Buf("sca")
        BETA, G, GC, GLT, GLA, GLB, BK, EKD = range(8)
        vecs = A.alloc([128, 3, 16], F32, "vecs")
        normw_bc = A.alloc([128, 128], F32, "normw_bc")
        cw = A.alloc([128, 32, 4], F32, "cw")
        Bv = Buf("vecs")
        P.dma(vecs[:, 0, :], AP(gdn_dtb.tensor, 0, [[0, 128], [1, 16]]), writes=[Bv])
        P.dma(vecs[:, 1, :], AP(gdn_alog.tensor, 0, [[0, 128], [1, 16]]), writes=[Bv])
        P.dma(normw_bc[:], AP(gdn_normw.tensor, 0, [[0, 128], [1, 128]]), writes=[Bv])
        P.dma(cw[:], gdn_cw, writes=[Bv])
        P.actv(vecs[:, 1, :], vecs[:, 1, :], AF.Exp, reads=[Bv], writes=[Bv])
        P.ts("dve", vecs[:, 1, :], vecs[:, 1, :], -1.0, None, ALU.mult, reads=[Bv], writes=[Bv])
        m1 = A.mark()
        wba = A.alloc([128, 8, 32], BF16, "wba")
        Bw = Buf()
        P.dma(wba[:], gdn_w_in[:, 6144:6176].rearrange("(kc p) n -> p kc n", p=128), writes=[Bw], q="pool")
        lin = A.alloc([128, NT, 32], F32, "lin")
        Bl = Buf()
        for t in range(NT):
            pb = t % 2
            for kc in range(8):
                P.mm(psum[pb][:, 0:32], hT[:, kc, t * 128:(t + 1) * 128], wba[:, kc, :], start=(kc == 0), stop=(kc == 7),
                     reads=[BhT, Bw], writes=[Bps[pb]])
            P.copy("dve", lin[:, t, :], psum[pb][:, 0:32], reads=[Bps[pb]], writes=[Bl])
        P.actv(sca[:, BETA], lin[:, :, 0:16], AF.Sigmoid, reads=[Bl], writes=[Bsc])
        P.tt("dve", sca[:, G], lin[:, :, 16:32], bc(vecs[:, 0, :].unsqueeze(1), [128, NT, 16]), ALU.add, reads=[Bl, Bv], writes=[Bsc])
        P.actv(sca[:, G], sca[:, G], AF.Exp, reads=[Bsc], writes=[Bsc])
        P.actv(sca[:, G], sca[:, G], AF.Ln, reads=[Bsc], writes=[Bsc], bias=1.0)
        P.tt("dve", sca[:, G], sca[:, G], bc(vecs[:, 1, :].unsqueeze(1), [128, NT, 16]), ALU.mult, reads=[Bsc, Bv], writes=[Bsc])
        gflat = sca[:, G].rearrange("p t h -> p (t h)")
        for i, (nm, dst) in enumerate((("u64", GC), ("onesbd", GLT), ("ones_a", GLA), ("ones_b", GLB))):
            pb = i % 2
            P.mm(psum[pb][:, 0:256], C[nm], gflat, reads=[Bsc, Bcst], writes=[Bps[pb]])
            P.copy("dve", sca[:, dst].rearrange("p t h -> p (t h)"), psum[pb][:, 0:256], reads=[Bps[pb]], writes=[Bsc])
        P.tt("dve", sca[:, EKD], sca[:, GLT], sca[:, GC], ALU.subtract, reads=[Bsc], writes=[Bsc])
        P.actv(sca[:, EKD], sca[:, EKD], AF.Exp, reads=[Bsc], writes=[Bsc])
        P.actv(sca[:, BK], sca[:, GC], AF.Exp, reads=[Bsc], writes=[Bsc])
        P.tt("dve", sca[:, BK], sca[:, BK], sca[:, BETA], ALU.mult, reads=[Bsc], writes=[Bsc])
        P.actv(sca[:, GLA], sca[:, GLA], AF.Exp, reads=[Bsc], writes=[Bsc])
        P.actv(sca[:, GLB], sca[:, GLB], AF.Exp, reads=[Bsc], writes=[Bsc])
        P.ts("dve", sca[:, GLT], sca[:, BETA], -1.0, None, ALU.mult, reads=[Bsc], writes=[Bsc])
        NBETA = GLT
        tap("sca", sca[:], [Bsc])
        A.release(m1)
        ck("C0")

        mG = A.mark()
        wbf_ = [A.alloc([128, 8, 768], BF16, "wbf%d" % i) for i in range(2)]
        Bwbf_ = [Buf("wbf0"), Buf("wbf1")]

        def load_gdn_w(g_):
            cols_ = [(g_ * 128, 128, 0), (1024 + g_ * 128, 128, 128), (2048 + g_ * 256, 256, 256), (4096 + g_ * 256, 256, 512)]
            for (c0, n, o0) in cols_:
                P.dma(wbf_[g_ % 2][:, :, o0:o0 + n], gdn_w_in[:, c0:c0 + n].rearrange("(kc p) n -> p kc n", p=128),
                      writes=[Bwbf_[g_ % 2]], q="pool")

        load_gdn_w(0)
        if mode == "all":
            wsc = [A.alloc([128, 8, 128], F32, "wsc%d" % i) for i in range(2)]
            Bwsc = [Buf(), Buf()]
            stage_a_col(0, wsc, Bwsc, fcs=range(16, 24), with_sc1=False)
            stage_a_col(1, wsc, Bwsc)
        dg = A.alloc([128, 4, 4, 128], BF16, "dg")
        Bdg = Buf("dg")
        qkT_g = [A.alloc([128, 2, T], BF16, "qkT%d" % i) for i in range(2)]
        BqkT_g = [Buf(), Buf()]
        ktok_g = [A.alloc([128, NT, 128], BF16, "ktok%d" % i) for i in range(2)]
        Bktok_g = [Buf(), Buf()]
        vtok_g = [A.alloc([128, NT, 2, 128], BF16, "vtok%d" % i) for i in range(2)]
        Bvtok_g = [Buf(), Buf()]
        zs_g = [A.alloc([128, NT, 2, 128], BF16, "zs%d" % i) for i in range(2)]
        Bzs_g = [Buf(), Buf()]
        ogun_g = [A.alloc([128, NT, 2, 128], BF16, "ogun%d" % i) for i in range(2)]
        Bog_g = [Buf(), Buf()]
        ssq_g = [A.alloc([128, NT, 2], F32, "ssq%d" % i) for i in range(2)]
        Bssq_g = [Buf(), Buf()]
        pre_ = [A.alloc([128, 3 + T], BF16, "pre%d" % i) for i in range(2)]
        Bpre_ = [Buf("pre0"), Buf("pre1")]
        NS1 = 2
        s1 = [A.alloc([128, 512], F32, "s1_%d" % i) for i in range(NS1)]
        Bs1 = [Buf() for _ in range(NS1)]
        sq = [A.alloc([128, 512], BF16, "sq_%d" % i) for i in range(NS1)]
        Bsq = [Buf() for _ in range(NS1)]
        r1 = [A.alloc([128, 512], F32, "r1_%d" % i) for i in range(NS1)]
        Br1 = [Buf() for _ in range(NS1)]
        NB2 = 2
        RB = os.environ.get("KRB", "act")
        grr_ = [A.alloc([128, 2, 128], F32, "grr%d" % i) for i in range(NB2)]
        Bgrr_ = [Buf() for _ in range(NB2)]
        egr_ = [A.alloc([128, 2, 128], BF16, "egr%d" % i) for i in range(NB2)]
        Begr_ = [Buf() for _ in range(NB2)]
        a1_ = [A.alloc([128, 2, 128], F32, "a1_%d" % k) for k in range(NB2)]
        a2_ = [A.alloc([128, 2, 128], F32, "a2_%d" % k) for k in range(NB2)]
        Ba1_ = [Buf() for _ in range(NB2)]
        Ba2_ = [Buf() for _ in range(NB2)]
        X_ = [[A.alloc([128, 2, 128], neumann_dt, "X%d_%d" % (i, k)) for i in range(2)] for k in range(NB2)]
        XP_ = [[A.alloc([128, 2, 2, 128], neumann_dt, "XP%d_%d" % (i, k)) for i in range(2)] for k in range(NB2)]
        BX_ = [[Buf(), Buf()] for _ in range(NB2)]
        BXP_ = [[Buf(), Buf()] for _ in range(NB2)]
        TT_ = [A.alloc([128, 2, 128], BF16, "TT%d" % i) for i in range(NB2)]
        BTT_ = [Buf() for _ in range(NB2)]
        QKm_ = [A.alloc([128, 2, 128], BF16, "QKm%d" % i) for i in range(NB2)]
        BQKm_ = [Buf() for _ in range(NB2)]
        kb_ = [A.alloc([128, 2, 128], BF16, "kb%d" % i) for i in range(NB2)]
        vb_ = [A.alloc([128, 2, 128], BF16, "vb%d" % i) for i in range(NB2)]
        kdec_ = [A.alloc([128, 2, 128], BF16, "kdec%d" % i) for i in range(NB2)]
        qd_ = [A.alloc([128, 2, 128], BF16, "qd%d" % i) for i in range(NB2)]
        Bkb_, Bvb_, Bkdec_, Bqd_ = [[Buf() for _ in range(NB2)] for _ in range(4)]
        negwT_ = [A.alloc([128, 2, 128], BF16, "negwT%d" % i) for i in range(NB2)]
        BnegwT_ = [Buf() for _ in range(NB2)]
        vnew = A.alloc([128, 2, 128], BF16, "vnew")
        Bvnew = [Buf(), Buf()]
        S32 = A.alloc([128, 2, 128], F32, "S32")
        BS32 = [Buf(), Buf()]
        Sbf = [A.alloc([128, 2, 128], BF16, "Sbf%d" % i) for i in range(2)]
        BSbf = [[Buf(), Buf()], [Buf(), Buf()]]
        identN = C["ident"]
        if neumann_dt != F32:
            identN_t = A.alloc([128, 128], neumann_dt, "identN")
            P.copy("dve", identN_t[:], C["ident"], reads=[Bcst], writes=[Bcst])
            identN = identN_t[:]

        BpKK, BpW, BpGR, BpT, BpA, BpB, BpV, BpO = Bps
        BpS = BpV
        psKK = psum[0][:, 0:256]
        psW = psum[1][:, 0:256].rearrange("p (h n) -> p h n", h=2)
        psGR = psum[2][:, 0:256].rearrange("p (h n) -> p h n", h=2)
        psT = psum[3][:, 0:256].rearrange("p (h n) -> p h n", h=2)
        psA = psum[4][:].rearrange("p (h n) -> p h n", h=2)
        psB = psum[5][:, 0:256].rearrange("p (h n) -> p h n", h=2)
        psV = psum[6][:, 0:256].rearrange("p (h n) -> p h n", h=2)
        psS = psum[6][:, 256:512].rearrange("p (h n) -> p h n", h=2)
        psO = psum[7][:, 0:256].rearrange("p (h n) -> p h n", h=2)
        evq = [0]

        def ev_eng():
            evq[0] += 1
            return "act" if evq[0] % 2 else "dve"

        for hq in range(8):
            cols = [(hq * 128, 128, 0), (1024 + hq * 128, 128, 128), (2048 + hq * 256, 256, 256), (4096 + hq * 256, 256, 512)]
            gp = hq % 2
            qkT, BqkT, ktok, Bktok = qkT_g[gp], BqkT_g[gp], ktok_g[gp], Bktok_g[gp]
            vtok, Bvtok, zs, Bzs = vtok_g[gp], Bvtok_g[gp], zs_g[gp], Bzs_g[gp]
            ogun, Bog, ssq, Bssq = ogun_g[gp], Bog_g[gp], ssq_g[gp], Bssq_g[gp]
            wbf, Bwbf = wbf_[hq % 2], Bwbf_[hq % 2]
            for pi_ in range(2):
                P.op("pool", lambda e, pi_=pi_: e.memset(pre_[pi_][:, 0:3], 0.0), writes=[Bpre_[pi_]])
            prec = [0]
            chunks = [hq, 8 + hq, 16 + 2 * hq, 17 + 2 * hq]
            for ci, ch in enumerate(chunks):
                for j in range(4):
                    P.ts("dve", dg[:, ci, j, :], C["ident"], cw[:, ch, j:j + 1], None, ALU.mult, reads=[Bcst, Bv], writes=[Bdg])

            def inproj_fm(wc0):
                pre, Bpre = pre_[prec[0] % 2], Bpre_[prec[0] % 2]
                prec[0] += 1
                for tb in range(4):
                    pb = (1, 3)[tb % 2]
                    for kc in range(8):
                        P.mm(psum[pb][:, :], wbf[:, kc, wc0:wc0 + 128], hT[:, kc, tb * 512:(tb + 1) * 512],
                             start=(kc == 0), stop=(kc == 7), reads=[Bwbf, BhT], writes=[Bps[pb]])
                    P.copy(ev_eng(), pre[:, 3 + tb * 512:3 + (tb + 1) * 512], psum[pb][:, :], reads=[Bps[pb]], writes=[Bpre])
                return pre, Bpre

            for f in range(2):
                pre, Bpre = inproj_fm(f * 128)
                for tb in range(4):
                    i = (f * 4 + tb) % NS1
                    pb = (0, 2)[tb % 2]
                    for j in range(4):
                        P.mm(psum[pb][:, :], dg[:, f, j, :], pre[:, tb * 512 + j:tb * 512 + j + 512], start=(j == 0), stop=(j == 3),
                             reads=[Bdg, Bpre], writes=[Bps[pb]])
                    P.actv(s1[i][:], psum[pb][:, :], AF.Silu, reads=[Bps[pb]], writes=[Bs1[i]])
                    P.tt("pool", sq[i][:], s1[i][:], s1[i][:], ALU.mult, reads=[Bs1[i]], writes=[Bsq[i]])
                    P.mm(psum[pb][:, :], onesb[:], sq[i][:], reads=[Bsq[i], Bcst], writes=[Bps[pb]])
                    P.actv(r1[i][:], psum[pb][:, :], AF.Sqrt, reads=[Bps[pb]], writes=[Br1[i]], bias=EPS)
                    P.op("dve", lambda e, i=i: e.reciprocal(out=r1[i][:], in_=r1[i][:]), reads=[Br1[i]], writes=[Br1[i]])
                    sc = (128.0 ** -0.5) if f == 0 else 1.0
                    P.stt("dve", qkT[:, f, tb * 512:(tb + 1) * 512], s1[i][:], sc, r1[i][:], ALU.mult, ALU.mult,
                          reads=[Bs1[i], Br1[i]], writes=[BqkT])
            for t in range(NT):
                pb = (0, 2)[(t // 4) % 2]
                pst = psum[pb][:].bitcast(BF16)
                P.tr(pst[:, (t % 4) * 128:(t % 4 + 1) * 128], qkT[:, 1, t * 128:(t + 1) * 128], identb[:], reads=[BqkT, Bcst], writes=[Bps[pb]])
                if t % 4 == 3:
                    t0 = t - 3
                    P.copy(ev_eng(), ktok[:, t0:t0 + 4, :], pst[:, 0:512].rearrange("p (a b) -> p a b", a=4), reads=[Bps[pb]], writes=[Bktok])
            for vh in range(2):
                pre, Bpre = inproj_fm(256 + vh * 128)
                for t in range(NT):
                    pb = (0, 2)[(t // 4) % 2]
                    for j in range(4):
                        P.mm(psum[pb][:, (t % 4) * 128:(t % 4 + 1) * 128], pre[:, t * 128 + j:t * 128 + j + 128], dg[:, 2 + vh, j, :],
                             start=(j == 0), stop=(j == 3), reads=[Bdg, Bpre], writes=[Bps[pb]])
                    if t % 4 == 3:
                        t0 = t - 3
                        P.actv(vtok[:, t0:t0 + 4, vh, :], psum[pb][:, :].rearrange("p (a b) -> p a b", a=4), AF.Silu,
                               reads=[Bps[pb]], writes=[Bvtok])
            for t in range(NT):
                pb = (1, 3)[t % 2]
                for kc in range(8):
                    P.mm(psum[pb][:, 0:256], hT[:, kc, t * 128:(t + 1) * 128], wbf[:, kc, 512:768], start=(kc == 0), stop=(kc == 7),
                         reads=[Bwbf, BhT], writes=[Bps[pb]])
                i = t % NS1
                P.actv(s1[i][:, 0:256], psum[pb][:, 0:256], AF.Silu, reads=[Bps[pb]], writes=[Bs1[i]])
                P.tt("pool", zs[:, t, :, :], s1[i][:, 0:256].rearrange("p (a b) -> p a b", a=2),
                     bc(normw_bc[:].unsqueeze(1), [128, 2, 128]), ALU.mult, reads=[Bs1[i], Bv], writes=[Bzs])

            if hq == 0:
                tap("qkT", qkT[:], [BqkT]); tap("ktok", ktok[:], [Bktok]); tap("vtok", vtok[:], [Bvtok]); tap("zs", zs[:], [Bzs])
            ck("C1")
            if hq + 1 < 8:
                load_gdn_w(hq + 1)
            for hl in range(2):
                P.op("pool", lambda e, hl=hl: e.memset(S32[:, hl, :], 0.0), writes=[BS32[hl]])
                P.op("pool", lambda e, hl=hl: e.memset(Sbf[0][:, hl, :], 0.0), writes=[BSbf[0][hl]])
            sp = 0
            for t in range(NT):
                tl = slice(t * 128, (t + 1) * 128)
                h0 = 2 * hq
                k2 = t % NB2
                grr, Bgrr, egr, Begr = grr_[k2], Bgrr_[k2], egr_[k2], Begr_[k2]
                a1, a2, Ba1, Ba2 = a1_[k2], a2_[k2], Ba1_[k2], Ba2_[k2]
                TT, BTT, QKm, BQKm = TT_[k2], BTT_[k2], QKm_[k2], BQKm_[k2]
                kb, vb, kdec, qd = kb_[k2], vb_[k2], kdec_[k2], qd_[k2]
                Bkb, Bvb, Bkdec, Bqd = Bkb_[k2], Bvb_[k2], Bkdec_[k2], Bqd_[k2]
                negwT, BnegwT = negwT_[k2], BnegwT_[k2]
                X, XP, BX, BXP = X_[k2], XP_[k2], BX_[k2], BXP_[k2]
                P.tt("dve", grr[:], bc(C["ident"].unsqueeze(1), [128, 2, 128]),
                     bc(sca[:, GC, t, h0:h0 + 2].unsqueeze(2), [128, 2, 128]), ALU.mult, reads=[Bcst, Bsc], writes=[Bgrr])
                P.mm(psum[2][:, 0:256], C["ones"], grr[:].rearrange("p a b -> p (a b)"), reads=[Bcst, Bgrr], writes=[BpGR])
                P.actv(egr[:], psGR, AF.Exp, reads=[BpGR], writes=[Begr])
                P.mm(psKK[:, 0:128], qkT[:, 1, tl], qkT[:, 1, tl], reads=[BqkT], writes=[BpKK])
                P.mm(psKK[:, 128:256], qkT[:, 1, tl], qkT[:, 0, tl], reads=[BqkT], writes=[BpKK])
                ck("C2a")
                cur = 0
                for hl in range(2):
                    h = h0 + hl
                    P.stt("dve", a1[:, hl, :], psGR[:, hl, :], sca[:, GC, t, h:h + 1], C["mask_a"], ALU.subtract, ALU.max,
                          reads=[BpGR, Bsc, Bcst], writes=[Ba1])
                    P.stt("dve", a2[:, hl, :], psGR[:, hl, :], sca[:, GC, t, h:h + 1], C["mask_q"], ALU.subtract, ALU.min,
                          reads=[BpGR, Bsc, Bcst], writes=[Ba2])
                P.actv(a1[:], a1[:], AF.Exp, reads=[Ba1], writes=[Ba1], scale=-1.0)
                P.actv(a2[:], a2[:], AF.Exp, reads=[Ba2], writes=[Ba2])
                for hl in range(2):
                    h = h0 + hl
                    P.stt("dve", X[cur][:, hl, :], psKK[:, 0:128], sca[:, NBETA, t, h:h + 1], a1[:, hl, :], ALU.mult, ALU.mult,
                          reads=[BpKK, Bsc, Ba1], writes=[BX[cur]])
                    P.tt("dve", QKm[:, hl, :], psKK[:, 128:256], a2[:, hl, :], ALU.mult, reads=[BpKK, Ba2], writes=[BQKm])
                    P.tr(psT[:, hl, :], X[cur][:, hl, :], identN, reads=[BX[cur], Bcst], writes=[BpT])
                ck("D4")
                P.copy("act", XP[cur][:, :, 0, :], psT, reads=[BpT], writes=[BXP[cur]])
                ck("D5")
                P.copy("dve", XP[cur][:, :, 1, :], bc(identN.unsqueeze(1), [128, 2, 128]), reads=[Bcst], writes=[BXP[cur]])
                ck("C2b")
                for hl in range(2):
                    h = h0 + hl
                    P.ts("act", kb[:, hl, :], ktok[:, t, :], sca[:, BK, t, h:h + 1], None, ALU.mult, reads=[Bktok, Bsc], writes=[Bkb])
                    P.ts("act", vb[:, hl, :], vtok[:, t, hl, :], sca[:, BETA, t, h:h + 1], None, ALU.mult, reads=[Bvtok, Bsc], writes=[Bvb])
                    P.ts("pool", kdec[:, hl, :], ktok[:, t, :], sca[:, EKD, t, h:h + 1], None, ALU.mult, reads=[Bktok, Bsc], writes=[Bkdec])
                P.tt("pool", qd[:], bc(qkT[:, 0, tl].unsqueeze(1), [128, 2, 128]), egr[:], ALU.mult, reads=[BqkT, Begr], writes=[Bqd])
                ck("C2c")
                for k in range(6):
                    nxt = 1 - cur
                    last = (k == 5)
                    for hl in range(2):
                        if not last:
                            P.mm(psA[:, hl, :], X[cur][:, hl, :], XP[cur][:, hl, :, :].rearrange("p a b -> p (a b)"),
                                 reads=[BX[cur], BXP[cur]], writes=[BpA])
                            P.mm(psB[:, hl, :], XP[cur][:, hl, 0, :], X[cur][:, hl, :], reads=[BX[cur], BXP[cur]], writes=[BpB])
                        else:
                            P.mm(psA[:, hl, 128:256], X[cur][:, hl, :], XP[cur][:, hl, 1, :], reads=[BX[cur], BXP[cur]], writes=[BpA])
                    if not last:
                        P.copy("act", XP[nxt][:, :, 0, :], psA[:, :, 0:128], reads=[BpA], writes=[BXP[nxt]])
                        P.tt("dve", XP[nxt][:, :, 1, :], XP[cur][:, :, 1, :], psA[:, :, 128:256], ALU.add, reads=[BpA, BXP[cur]], writes=[BXP[nxt]])
                        P.copy("dve" if k % 2 == 0 else "act", X[nxt][:], psB, reads=[BpB], writes=[BX[nxt]])
                    else:
                        P.tt("dve", TT[:], XP[cur][:, :, 1, :], psA[:, :, 128:256], ALU.add, reads=[BpA, BXP[cur]], writes=[BTT])
                    cur = nxt
                ck("C2d")
                if hq == 0 and t == 0:
                    tap("TT", TT[:], [BTT]); tap("QKm", QKm[:], [BQKm]); tap("a1", a1[:, 0, :], [Ba1]); tap("a2", a2[:, 0, :], [Ba2])
                for hl in range(2):
                    P.mm(psW[:, hl, :], kb[:, hl, :], TT[:, hl, :], reads=[Bkb, BTT], writes=[BpW])
                P.actv(negwT[:], psW, AF.Copy, reads=[BpW], writes=[BnegwT], scale=-1.0)
                ck("C2e")
                for cb in range(2):
                    rows = slice(cb * 64, cb * 64 + 64)
                    tp = (0, 64 * cb)
                    tpk = (64 * cb, 64 * cb)
                    for hl in range(2):
                        h = h0 + hl
                        P.mm(psV[rows, hl, :], TT[:, hl, rows], vb[:, hl, :], start=True, stop=False,
                             reads=[BTT, Bvb], writes=[BpV], tile_position=tp)
                        P.mm(psV[rows, hl, :], negwT[:, hl, rows], Sbf[sp][:, hl, :], start=False, stop=True,
                             reads=[BnegwT, BSbf[sp][hl]], writes=[BpV], tile_position=tp)
                    P.copy("act" if cb == 0 else "dve", vnew[rows, :, :], psV[rows, :, :], reads=[BpV], writes=[Bvnew[0], Bvnew[1]])
                    for hl in range(2):
                        P.mm(psS[:, hl, :], kdec[rows, hl, :], vnew[rows, hl, :], reads=[Bkdec, Bvnew[hl]], writes=[BpS],
                             tile_position=(64 * cb, 0))
                    for hl in range(2):
                        P.mm(psO[rows, hl, :], qd[:, hl, rows], Sbf[sp][:, hl, :], start=True, stop=False,
                             reads=[Bqd, BSbf[sp][hl]], writes=[BpO], tile_position=tp)
                        P.mm(psO[rows, hl, :], QKm[rows, hl, rows], vnew[rows, hl, :], start=False, stop=True,
                             reads=[BQKm, Bvnew[hl]], writes=[BpO], tile_position=tpk)
                    for hl in range(2):
                        h = h0 + hl
                        eg = sca[:, GLA if cb == 0 else GLB, t, h:h + 1]
                        P.stt("dve", Sbf[1 - sp][:, hl, :], S32[:, hl, :], eg, psS[:, hl, :], ALU.mult, ALU.add,
                              reads=[BS32[hl], Bsc, BpS], writes=[BSbf[1 - sp][hl]])
                        P.stt("dve", S32[:, hl, :], S32[:, hl, :], eg, psS[:, hl, :], ALU.mult, ALU.add,
                              reads=[BS32[hl], Bsc, BpS], writes=[BS32[hl]])
                    sp = 1 - sp
                    ck("C2f%d" % cb)
                for hl in range(2):
                    P.actv(a1[:, hl, :], psO[:, hl, :], AF.Square, reads=[BpO], writes=[Ba1, Bssq], accum_out=ssq[:, t, hl:hl + 1])
                P.tt("dve", ogun[:, t, :, :], psO, zs[:, t, :, :], ALU.mult, reads=[BpO, Bzs], writes=[Bog])
                ck("C2")
            if hq == 0:
                tap("ogun", ogun[:], [Bog]); tap("ssq", ssq[:], [Bssq])
            ssf = ssq[:].rearrange("p t h -> p (t h)")
            P.actv(ssf, ssf, AF.Sqrt, reads=[Bssq], writes=[Bssq], scale=1.0 / 128, bias=EPS)
            P.op("dve", lambda e, ssf=ssf: e.reciprocal(out=ssf, in_=ssf), reads=[Bssq], writes=[Bssq])
            P.tt("pool", ogun[:].rearrange("p t h e -> p (t h) e"), ogun[:].rearrange("p t h e -> p (t h) e"),
                 bc(ssf.unsqueeze(2), [128, 2 * NT, 128]), ALU.mult, reads=[Bog, Bssq], writes=[Bog])
            P.dma(og_d.rearrange("(t p) (h e) -> p t h e", p=128, e=128)[:, :, 2 * hq:2 * hq + 2, :], ogun[:], reads=[Bog], q="sp")
            if hq == 7:
                wsrc = gdn_w_out.rearrange("(h e) n -> e h n", e=128)
                for hp in range(4):
                    P.dma(wo_alias[:, hp * 4:(hp + 1) * 4, :], wsrc[:, hp * 4:(hp + 1) * 4, :], writes=[BhT], q="pool")
            ck("C3")
        A.release(mG)
        ck("C4")

        if mode == "all":
            make_gate_bc(0)
            nsa_consts()
        mO = A.mark()
        wo, Bwo = wo_alias, BhT
        NO = 4
        xr = [A.alloc([128, D], F32, "xr%d" % i) for i in range(NO)]
        Bxr = [Buf() for _ in range(NO)]
        yo = [A.alloc([128, D], F32, "yo%d" % i) for i in range(NO)]
        Byo = [Buf() for _ in range(NO)]
        ogt = [A.alloc([128, 2048], BF16, "ogt%d" % i) for i in range(NO)]
        Bogt = [Buf() for _ in range(NO)]
        oTt = [A.alloc([128, 16, 128], BF16, "oTt%d" % i) for i in range(NO)]
        BoTt = [Buf() for _ in range(NO)]
        for t in range(NT):
            i = t % NO
            P.dma(xr[i][:], src_d[t * 128:(t + 1) * 128, :], writes=[Bxr[i]], q="sp")
            P.dma(ogt[i][:], og_d[t * 128:(t + 1) * 128, :], writes=[Bogt[i]], q="sp")
            for half in range(2):
                pb = 2 + half
                pst = psum[pb][:].bitcast(BF16)
                for h8 in range(8):
                    h = half * 8 + h8
                    P.tr(pst[:, h8 * 128:(h8 + 1) * 128], ogt[i][:, h * 128:(h + 1) * 128], identb[:], reads=[Bogt[i], Bcst], writes=[Bps[pb]])
                P.copy("act" if half == 0 else "dve", oTt[i][:, half * 8:(half + 1) * 8, :].rearrange("p a b -> p (a b)"), pst[:, 0:1024],
                       reads=[Bps[pb]], writes=[BoTt[i]])
            for hh in range(2):
                pb = hh
                for h in range(16):
                    P.mm(psum[pb][:, :], oTt[i][:, h, :], wo[:, h, hh * 512:(hh + 1) * 512],
                         start=(h == 0), stop=(h == 15), reads=[BoTt[i], Bwo], writes=[Bps[pb]])
                P.tt("dve", yo[i][:, hh * 512:(hh + 1) * 512], psum[pb][:, :], gate_bc[:, hh * 512:(hh + 1) * 512], ALU.mult,
                     reads=[Bps[pb], Bmod], writes=[Byo[i]])
            P.tt("pool", yo[i][:], yo[i][:], xr[i][:], ALU.add, reads=[Byo[i], Bxr[i]], writes=[Byo[i]])
            o = P.dma(dst_d[t * 128:(t + 1) * 128, :], yo[i][:], reads=[Byo[i]], q="sp")
            final_ops.append(o)
        A.release(mO)
        A.release(mL)

    NS = {}

    def nsa_consts():
        NS["top0"] = A.top
        kcmpT = A.alloc_top([128, 4, 128], BF16, "kcmpT")
        vcmp = A.alloc_top([128, 4, 97], F32, "vcmp")
        bias = A.alloc_top([128, 2, 16, 128], BF16, "bias")
        gsm = A.alloc_top([128, 16, 128], BF16, "gsm")
        shb = A.alloc_top([128, NT, 127], BF16, "shb")
        emb = A.alloc_top([128, NT, 128], BF16, "emb")
        wmb = A.alloc_top([128, 128], BF16, "wmb")
        cmask = A.alloc_top([128, NT, 32], F32, "cmask")
        fbias = A.alloc_top([128, NT, 32], F32, "fbias")
        fgb = A.alloc_top([128, D], F32, "fgb")
        Bkc, Bvc, Bbias, Bgsm, Bk1 = [Buf() for _ in range(5)]
        P.dma(cmask[:], nsa_cmask, writes=[Bk1])
        P.dma(fbias[:], nsa_fbias, writes=[Bk1])
        P.dma(fgb[:], AP(final_g.tensor, 0, [[0, 128], [1, D]]), writes=[Bk1])
        P.op("pool", lambda e: e.memset(vcmp[:], 1.0), writes=[Bvc])
        P.dma(vcmp[0:127, :, 65:97], AP(nsa_ovl.tensor, 0, [[32, 127], [0, 4], [1, 32]]), writes=[Bvc], q="pool")
        relb = A.alloc([33, 16], F32, "relb")
        ohs = A.alloc([33, 384], F32, "ohs")
        tbl = A.alloc([16, 384], F32, "tbl")
        Bt0, Bt1, BZ = Buf(), Buf(), Buf()
        P.op("pool", lambda e: e.memset(relb[:], 1.0), writes=[Bt0])
        P.dma(relb[0:32, :], rel_bias_d, writes=[Bt0])
        P.dma(ohs[:], nsa_oh, writes=[Bt0])
        P.mm(psum[3][0:16, 0:384], relb[:, :], ohs[:, :], reads=[Bt0], writes=[Bps[3]])
        P.copy("dve", tbl[:], psum[3][0:16, 0:384], reads=[Bps[3]], writes=[Bt1])
        WD = 400
        P.dma(AP(zt_d.tensor, 0, [[128 * WD, 16], [WD, 128], [1, 384]]), bc(tbl[:].unsqueeze(1), [16, 128, 384]),
              reads=[Bt1], writes=[BZ])
        P.op("pool", lambda e: e.memset(gsm[:], 0.0), writes=[Bgsm])
        P.op("pool", lambda e: e.memset(gsm[0:16, :, :], -BIG), writes=[Bgsm])
        for h in range(16):
            for dl in range(2):
                P.dma(bias[:, dl, h, :], AP(zt_d.tensor, h * 128 * WD + 128 * (dl + 1), [[WD - 1, 128], [1, 128]]),
                      reads=[BZ], writes=[Bbias], q="pool")
            P.dma(gsm[0:15, h, :], AP(zt_d.tensor, h * 128 * WD + 225, [[WD - 16, 15], [1, 128]]), reads=[BZ], writes=[Bgsm], q="pool")
        P.dma(shb[:], nsa_sh, writes=[Bk1], q="pool")
        P.dma(emb[:], nsa_em, writes=[Bk1], q="pool")
        P.dma(wmb[:], nsa_wm, writes=[Bk1], q="pool")
        NS.update(kcmpT=kcmpT, vcmp=vcmp, bias=bias, gsm=gsm, shb=shb, emb=emb, wmb=wmb, cmask=cmask, fbias=fbias, fgb=fgb,
                  Bkc=Bkc, Bvc=Bvc, Bbias=Bbias, Bgsm=Bgsm, Bk1=Bk1)

    def layer_nsa(src_d, dst_d):
        l = 1
        mL = A.mark()
        if not NS:
            nsa_consts()
            A.release(mL)
        top0 = NS["top0"]
        kcmpT, vcmp, bias, gsm, shb, emb, wmb = NS["kcmpT"], NS["vcmp"], NS["bias"], NS["gsm"], NS["shb"], NS["emb"], NS["wmb"]
        cmask, fbias, fgb = NS["cmask"], NS["fbias"], NS["fgb"]
        Bkc, Bvc, Bbias, Bgsm, Bk1 = NS["Bkc"], NS["Bvc"], NS["Bbias"], NS["Bgsm"], NS["Bk1"]
        BqT, BksT, BkwT, Bvs, Bvw, Bzsn, Bgts = [Buf() for _ in range(7)]
        ck("N0")

        mP = A.mark()
        hT = A.alloc([128, 8, T], BF16, "hT1")
        BhT = Buf("hT1")
        wbf = [A.alloc([128, 8, 512], BF16, "nwbf%d" % i) for i in range(2)]
        Bwbf = [Buf(), Buf()]
        wcnt = [0]
        evq = [0]

        def ev_eng():
            evq[0] += 1
            return "act" if evq[0] % 2 else "dve"

        def load_w(c0, n):
            i = wcnt[0] % 2
            wcnt[0] += 1
            for kh in range(2):
                P.dma(wbf[i][:, kh * 4:(kh + 1) * 4, 0:n], nsa_w[kh * 512:(kh + 1) * 512, c0:c0 + n].rearrange("(kc p) n -> p kc n", p=128),
                      writes=[Bwbf[i]], q="pool")
            return wbf[i], Bwbf[i]

        pre_w = [load_w(2048, 512), load_w(0, 512)]
        run_stage_a([0, 1])
        make_hT(src_d, l, hT, BhT)
        A.off = stage_a_mark[0] if stage_a_mark[0] is not None else A.off
        make_gate_bc(l)
        ck("N0b")

        pcnt = [0]

        def proj_fm(w, Bw, wc0, dst, Bdst, scale=None):
            for tb in range(4):
                pb = pcnt[0] % 2
                pcnt[0] += 1
                for kc in range(8):
                    P.mm(psum[pb][:, :], w[:, kc, wc0:wc0 + 128], hT[:, kc, tb * 512:(tb + 1) * 512],
                         start=(kc == 0), stop=(kc == 7), reads=[Bw, BhT], writes=[Bps[pb]])
                e = ev_eng()
                if scale is None:
                    P.copy(e, dst[:, tb * 512:(tb + 1) * 512], psum[pb][:, :], reads=[Bps[pb]], writes=[Bdst])
                elif e == "act":
                    P.actv(dst[:, tb * 512:(tb + 1) * 512], psum[pb][:, :], AF.Copy, reads=[Bps[pb]], writes=[Bdst], scale=scale)
                else:
                    P.ts("dve", dst[:, tb * 512:(tb + 1) * 512], psum[pb][:, :], scale, None, ALU.mult, reads=[Bps[pb]], writes=[Bdst])

        CQ, CKS, CKW, CKC, CVC, CVSW, CZ, CG = 0, 1024, 1536, 2048, 2304, 2560, 3072, 4096

        mC = A.mark()
        kvcT = A.alloc([128, 4, T], BF16, "kvcT")
        BkvcT = Buf()
        w, Bw = pre_w[0]
        for c in range(4):
            proj_fm(w, Bw, c * 128, kvcT[:, c, :], BkvcT)
        w1b = A.alloc([128, 32, 64], BF16, "w1b")
        w2b = A.alloc([64, 128], BF16, "w2b")
        posb = A.alloc([128, 32], BF16, "posb")
        c1 = A.alloc([64, 1], F32, "c1")
        hid = A.alloc([64, 4, 128], BF16, "hid")
        Bw1, Bw2, Bpos, Bc1, Bhid = Buf(), Buf(), Buf(), Buf(), Buf()
        for br in range(2):
            for half in range(2):
                P.dma(w1b[half * 64:(half + 1) * 64, :, :], nsa_w1[br].rearrange("(l d) j -> d l j", d=64), writes=[Bw1], q="pool")
                P.dma(posb[half * 64:(half + 1) * 64, :], nsa_pos[br].rearrange("l d -> d l"), writes=[Bpos], q="pool",
                      allow_slow_non_contiguous=True)
            P.dma(w2b[:, 0:64], nsa_w2[br], writes=[Bw2], q="pool")
            P.dma(w2b[:, 64:128], nsa_w2[br], writes=[Bw2], q="pool")
            for lq in range(32):
                P.mm(psum[2][0:64, 0:1], w1b[0:64, lq, :], posb[0:64, lq:lq + 1], start=(lq == 0), stop=(lq == 31),
                     reads=[Bw1, Bpos], writes=[Bps[2]])
            P.copy("dve", c1[:], psum[2][0:64, 0:1], reads=[Bps[2]], writes=[Bc1])
            for g in range(4):
                half = g % 2
                rows = slice(half * 64, half * 64 + 64)
                src = kvcT[rows, br * 2 + g // 2, :]
                for lq in range(32):
                    P.mm(psum[3][0:64, 0:127], w1b[rows, lq, :], src[:, lq:lq + 16 * 126 + 1:16], start=(lq == 0), stop=(lq == 31),
                         reads=[Bw1, BkvcT], writes=[Bps[3]], tile_position=(half * 64, 0))
                P.actv(hid[:, g, 0:127], psum[3][0:64, 0:127], AF.Silu, reads=[Bps[3], Bc1], writes=[Bhid], bias=c1[:, 0:1])
                if br == 0:
                    P.mm(psum[2][:, 0:127], w2b[:, :], hid[:, g, 0:127], reads=[Bw2, Bhid], writes=[Bps[2]])
                    P.copy("dve", kcmpT[:, g, 0:127], psum[2][:, 0:127], reads=[Bps[2]], writes=[Bkc])
                else:
                    P.mm(psum[2][0:127, 0:64], hid[:, g, 0:127], w2b[:, 0:64], reads=[Bw2, Bhid], writes=[Bps[2]])
                    P.copy("dve", vcmp[0:127, g, 0:64], psum[2][0:127, 0:64], reads=[Bps[2]], writes=[Bvc])
        A.release(mC)
        ck("N1")
        qT = A.alloc_top([128, 8, T], BF16, "qT")
        ksT = A.alloc_top([128, 4, T], BF16, "ksT")
        kwT = A.alloc_top([128, 4, T], BF16, "kwT")
        vs_aug = A.alloc_top([128, NT, 4, 65], BF16, "vs_aug")
        vw_aug = A.alloc_top([128, NT, 4, 65], BF16, "vw_aug")
        zsn = A.alloc_top([128, NT, D], BF16, "zsn")
        gts = A.alloc_top([128, NT, 48], F32, "gts")
        P.op("pool", lambda e: e.memset(vs_aug[:], 1.0), writes=[Bvs])
        P.op("pool", lambda e: e.memset(vw_aug[:], 1.0), writes=[Bvw])

        for half2 in range(2):
            w, Bw = pre_w[1] if half2 == 0 else load_w(CQ + half2 * 512, 512)
            for c in range(4):
                proj_fm(w, Bw, c * 128, qT[:, half2 * 4 + c, :], BqT, scale=0.125)
        w, Bw = load_w(CKS, 512)
        for g in range(4):
            proj_fm(w, Bw, g * 128, ksT[:, g, :], BksT)
        w, Bw = load_w(CKW, 512)
        for g in range(4):
            proj_fm(w, Bw, g * 128, kwT[:, g, :], BkwT)
        w, Bw = load_w(CVSW, 512)
        for t in range(NT):
            pb = pcnt[0] % 2
            pcnt[0] += 1
            for kc in range(8):
                P.mm(psum[pb][:, :], hT[:, kc, t * 128:(t + 1) * 128], w[:, kc, :], start=(kc == 0), stop=(kc == 7),
                     reads=[Bw, BhT], writes=[Bps[pb]])
            P.copy("act", vs_aug[:, t, :, 0:64], psum[pb][:, 0:256].rearrange("p (g d) -> p g d", g=4), reads=[Bps[pb]], writes=[Bvs])
            P.copy("dve", vw_aug[:, t, :, 0:64], psum[pb][:, 256:512].rearrange("p (g d) -> p g d", g=4), reads=[Bps[pb]], writes=[Bvw])
        for zh in range(2):
            w, Bw = load_w(CZ + zh * 512, 512)
            for t in range(NT):
                pb = pcnt[0] % 2
                pcnt[0] += 1
                for kc in range(8):
                    P.mm(psum[pb][:, :], hT[:, kc, t * 128:(t + 1) * 128], w[:, kc, :], start=(kc == 0), stop=(kc == 7),
                         reads=[Bw, BhT], writes=[Bps[pb]])
                P.actv(zsn[:, t, zh * 512:(zh + 1) * 512], psum[pb][:, :], AF.Silu, reads=[Bps[pb]], writes=[Bzsn])
        w, Bw = load_w(CG, 48)
        for t in range(NT):
            pb = pcnt[0] % 2
            pcnt[0] += 1
            for kc in range(8):
                P.mm(psum[pb][:, 0:48], hT[:, kc, t * 128:(t + 1) * 128], w[:, kc, 0:48], start=(kc == 0), stop=(kc == 7),
                     reads=[Bw, BhT], writes=[Bps[pb]])
            P.copy("dve", gts[:, t, :], psum[pb][:, 0:48], reads=[Bps[pb]], writes=[Bgts])
        P.actv(gts[:], gts[:], AF.Sigmoid, reads=[Bgts], writes=[Bgts])
        A.release(mP)
        ck("N2")

        wo = A.alloc_top([128, 8, D], BF16, "nwo")
        Bwo = Buf()
        for ch in range(2):
            P.dma(wo[:, ch * 4:(ch + 1) * 4, :], nsa_wo[ch * 512:(ch + 1) * 512, :].rearrange("(c p) n -> p c n", p=128), writes=[Bwo], q="pool")
        NPT = 3
        PT = [A.alloc([128, 512], BF16, "PT%d" % i) for i in range(NPT)]
        BPT = [Buf() for _ in range(NPT)]
        ptc = [0]
        PTf = A.alloc([128, 2, 512], F32, "PTf")
        BPTf = [Buf() for _ in range(2)]
        accc = A.alloc([128, 4, 4, 97], F32, "accc")
        Baccc = Buf()
        accw = A.alloc([128, 4, 4, 65], F32, "accw")
        Baccw = [Buf() for _ in range(4)]
        accs = [A.alloc([128, 4, 65], F32, "accs%d" % i) for i in range(2)]
        Baccs = [Buf(), Buf()]
        imp = A.alloc([128, 4, 32], F32, "imp")
        mx8 = A.alloc([128, 4, 8], F32, "mx8")
        selb = A.alloc([128, 4, 32], BF16, "selb")
        selbT = A.alloc([128, 4, 128], BF16, "selbT")
        Bimp, Bselb, BselbT = Buf(), Buf(), [Buf() for _ in range(4)]
        P.op("pool", lambda e: e.memset(selbT[:], 0.0), writes=BselbT)
        rec = A.alloc([128, 3, 16], F32, "rec")
        Brec = Buf()
        recsw = A.alloc([128, 2, 4], F32, "recsw")
        Brecsw = Buf()
        tmpo = A.alloc([128, 2, 4, 64], F32, "tmpo")
        Btmpo = Buf()
        og = A.alloc([128, D], BF16, "og")
        Bog = Buf()
        ogT = A.alloc([128, 8, 128], BF16, "ogT")
        BogT = Buf()
        yo = A.alloc([128, D], F32, "nyo")
        Byo = Buf()
        fst = A.alloc([128, NT, 3], F32, "fst")
        Bfst = Buf()
        junk = tmpo[:].rearrange("p a b c -> p (a b c)").bitcast(BF16)
        impt = tmpo[:].rearrange("p a b c -> p (a b c)").rearrange("p (g b s) -> p g b s", g=4, b=4)
        scnt = [0]
        TINY = 1e-30

        qpad = [A.alloc([128, 512], BF16, "qpad%d" % g) for g in range(4)]
        Bqpad = [Buf() for _ in range(4)]
        for g in range(4):
            P.op("pool", lambda e, g=g: e.memset(qpad[g][:], 0.0), writes=[Bqpad[g]])

        def build_qpad(i, g):
            qs = slice(i * 128, (i + 1) * 128)
            P.copy("pool", qpad[g][0:64, 0:256].rearrange("p (a b) -> p a b", a=2), qT[0:64, 2 * g:2 * g + 2, qs], reads=[BqT], writes=[Bqpad[g]])
            P.copy("dve", qpad[g][64:128, 256:512].rearrange("p (a b) -> p a b", a=2), qT[64:128, 2 * g:2 * g + 2, qs], reads=[BqT], writes=[Bqpad[g]])

        def scores(i, g, kT_t, Bk, ncols_k, kslice, extra):
            pb = scnt[0] % 4
            scnt[0] += 1
            P.mm(psum[pb][0:ncols_k, 0:512], kT_t[:, g, kslice], qpad[g][:, :], start=True, stop=(len(extra) == 0),
                 reads=[Bk, Bqpad[g]], writes=[Bps[pb]])
            for n, (lt, rh, rd) in enumerate(extra):
                P.mm(psum[pb][0:ncols_k, 0:512], lt, rh, start=False, stop=(n == len(extra) - 1), reads=rd, writes=[Bps[pb]])
            return pb

        def branch_tiles(i, g, br):
            kT_t, Bk, v_aug, Bv = (ksT, BksT, vs_aug, Bvs) if br == 1 else (kwT, BkwT, vw_aug, Bvw)
            bank = 6 if br == 1 else 5
            j0 = 0 if br == 1 else max(0, i - 4)
            first = True
            for j in range(j0, i + 1):
                dl = i - j
                extra = []
                if br == 1 and j != i and i >= 4:
                    extra.append((emb[:, j, :], bc(selbT[:, g, :].unsqueeze(1), [128, 4, 128]), [Bk1, BselbT[g]]))
                if dl <= 1:
                    extra.append((identb[:], bias[:, dl, 4 * g:4 * g + 4, :].rearrange("p a b -> p (a b)"), [Bcst, Bbias]))
                if br == 2 and dl == 4:
                    extra.append((identb[:], bc(wmb[:].unsqueeze(1), [128, 4, 128]), [Bcst, Bk1]))
                pb = scores(i, g, kT_t, Bk, 128, slice(j * 128, (j + 1) * 128), extra)
                pi = ptc[0] % NPT
                ptc[0] += 1
                P.actv(PT[pi][:], psum[pb][:, :], AF.Exp, reads=[Bps[pb]], writes=[BPT[pi]])
                for b in range(4):
                    P.mm(psum[bank][:, b * 65:(b + 1) * 65], PT[pi][:, b * 128:(b + 1) * 128], v_aug[:, j, g, :],
                         start=first, stop=(j == i), reads=[BPT[pi], Bv], writes=[Bps[bank]], skip_group_check=True)
                    first = False
            if br == 1:
                P.copy("act", accs[g % 2][:], psum[bank][:, 0:260].rearrange("p (a b) -> p a b", a=4), reads=[Bps[bank]], writes=[Baccs[g % 2]])
            else:
                P.copy("dve", accw[:, g, :, :], psum[bank][:, 0:260].rearrange("p (a b) -> p a b", a=4), reads=[Bps[bank]], writes=[Baccw[g]])

        for i in range(NT):
            qs = slice(i * 128, (i + 1) * 128)
            P.dma(yo[:], src_d[i * 128:(i + 1) * 128, :], writes=[Byo], q="pool")
            for g in range(4):
                build_qpad(i, g)
            for gp in range(2):
                for g in (2 * gp, 2 * gp + 1):
                    extra = [(shb[:, i, :], gsm[:, 4 * g:4 * g + 4, :].rearrange("p a b -> p (a b)"), [Bk1, Bgsm])]
                    pb = scores(i, g, kcmpT, Bkc, 127, slice(0, 127), extra)
                    P.actv(PTf[0:127, g % 2, :], psum[pb][0:127, :], AF.Exp, reads=[Bps[pb]], writes=[BPTf[g % 2]])
                if gp == 0:
                    ck("N3")
                    branch_tiles(i, 0, 2)
                    branch_tiles(i, 1, 2)
                    ck("N4")
                else:
                    branch_tiles(i, 2, 2)
                    branch_tiles(i, 3, 2)
                for g in (2 * gp, 2 * gp + 1):
                    for b in range(4):
                        P.mm(psum[4][:, b * 97:(b + 1) * 97], PTf[0:127, g % 2, b * 128:(b + 1) * 128], vcmp[0:127, g, :],
                             start=(b == 0), stop=True, reads=[BPTf[g % 2], Bvc], writes=[Bps[4]], skip_group_check=True)
                    P.copy("dve", accc[:, g, :, :], psum[4][:, 0:388].rearrange("p (a b) -> p a b", a=4), reads=[Bps[4]], writes=[Baccc])
            P.ts("dve", rec[:, 0, :], accc[:, :, :, 64].rearrange("p g b -> p (g b)"), TINY, None, ALU.add, reads=[Baccc], writes=[Brec])
            P.op("dve", lambda e: e.reciprocal(out=rec[:, 0, :], in_=rec[:, 0, :]), reads=[Brec], writes=[Brec])
            if i >= 4:
                n_before = len(P.segs[-1])
                P.tt("dve", impt, accc[:, :, :, 65:97], bc(rec[:, 0, :].rearrange("p (g b) -> p g b", g=4).unsqueeze(3), [128, 4, 4, 32]),
                     ALU.mult, reads=[Baccc, Brec], writes=[Btmpo])
                P.tt("dve", imp[:], impt[:, :, 0, :], impt[:, :, 1, :], ALU.add, reads=[Btmpo], writes=[Bimp])
                P.tt("dve", imp[:], imp[:], impt[:, :, 2, :], ALU.add, reads=[Bimp, Btmpo], writes=[Bimp])
                P.tt("dve", imp[:], imp[:], impt[:, :, 3, :], ALU.add, reads=[Bimp, Btmpo], writes=[Bimp])
                P.tt("dve", imp[:], imp[:], bc(cmask[:, i, :].unsqueeze(1), [128, 4, 32]), ALU.mult, reads=[Bimp, Bk1], writes=[Bimp])
                P.tt("dve", imp[:], imp[:], bc(fbias[:, i, :].unsqueeze(1), [128, 4, 32]), ALU.add, reads=[Bimp, Bk1], writes=[Bimp])
                for g in range(4):
                    P.op("dve", lambda e, g=g: e.max(out=mx8[:, g, :], in_=imp[:, g, :]), reads=[Bimp], writes=[Bimp])
                    P.ts("dve", selb[:, g, :], imp[:, g, :], mx8[:, g, 7:8], -BIG, ALU.is_lt, ALU.mult, reads=[Bimp], writes=[Bselb])
                pst = psum[7][:].bitcast(BF16)
                for g in range(4):
                    P.tr(pst[0:32, g * 128:(g + 1) * 128], selb[:, g, :], identb[:], reads=[Bselb, Bcst], writes=[Bps[7]])
                for g in range(4):
                    P.copy("act" if g % 2 else "dve", selbT[0:32, g, :], pst[0:32, g * 128:(g + 1) * 128], reads=[Bps[7]], writes=[BselbT[g]])
                for o_ in P.segs[-1][n_before:]:
                    if o_.eng != "pe":
                        o_.cost *= 4.0
            ck("N5")
            P.tt("dve", rec[:, 0, :], rec[:, 0, :], gts[:, i, 0:16], ALU.mult, reads=[Brec, Bgts], writes=[Brec])
            for g in range(4):
                branch_tiles(i, g, 1)
                gi = g % 2
                P.ts("dve", recsw[:, 0, :], accs[gi][:, :, 64], TINY, None, ALU.add, reads=[Baccs[gi]], writes=[Brecsw])
                P.ts("dve", recsw[:, 1, :], accw[:, g, :, 64], TINY, None, ALU.add, reads=[Baccw[g]], writes=[Brecsw])
                P.op("dve", lambda e: e.reciprocal(out=recsw[:], in_=recsw[:]), reads=[Brecsw], writes=[Brecsw])
                P.tt("dve", recsw[:], recsw[:], gts[:, i, 16:48].rearrange("p (r h) -> p r h", r=2)[:, :, 4 * g:4 * g + 4], ALU.mult,
                     reads=[Brecsw, Bgts], writes=[Brecsw])
                P.tt("pool", tmpo[:, 0, :, :], accc[:, g, :, 0:64], bc(rec[:, 0, 4 * g:4 * g + 4].unsqueeze(2), [128, 4, 64]), ALU.mult,
                     reads=[Baccc, Brec], writes=[Btmpo])
                for r in range(2):
                    src_, Bsrc_ = (accs[gi][:, :, 0:64], Baccs[gi]) if r == 0 else (accw[:, g, :, 0:64], Baccw[g])
                    P.tt("pool", tmpo[:, 1, :, :], src_, bc(recsw[:, r, :].unsqueeze(2), [128, 4, 64]), ALU.mult,
                         reads=[Bsrc_, Brecsw], writes=[Btmpo])
                    P.tt("pool", tmpo[:, 0, :, :], tmpo[:, 0, :, :], tmpo[:, 1, :, :], ALU.add, reads=[Btmpo], writes=[Btmpo])
                P.tt("pool", og[:, g * 256:(g + 1) * 256], tmpo[:, 0, :, :].rearrange("p h d -> p (h d)"), zsn[:, i, g * 256:(g + 1) * 256],
                     ALU.mult, reads=[Btmpo, Bzsn], writes=[Bog])
            ck("N6")
            pst = psum[7][:].bitcast(BF16)
            for c in range(8):
                P.tr(pst[:, c * 128:(c + 1) * 128], og[:, c * 128:(c + 1) * 128], identb[:], reads=[Bog, Bcst], writes=[Bps[7]])
            P.copy("act", ogT[:].rearrange("p a b -> p (a b)"), pst[:, 0:1024], reads=[Bps[7]], writes=[BogT])
            for hh in range(2):
                pb = 7
                for c in range(8):
                    P.mm(psum[pb][:, :], ogT[:, c, :], wo[:, c, hh * 512:(hh + 1) * 512], start=(c == 0), stop=(c == 7),
                         reads=[BogT, Bwo], writes=[Bps[pb]])
                tview = tmpo[:].rearrange("p a b c -> p (a b c)")
                P.tt("dve", tview, psum[pb][:, :], gate_bc[:, hh * 512:(hh + 1) * 512], ALU.mult,
                     reads=[Bps[pb], Bmod], writes=[Btmpo])
                P.tt("pool", yo[:, hh * 512:(hh + 1) * 512], yo[:, hh * 512:(hh + 1) * 512], tview, ALU.add, reads=[Btmpo, Byo], writes=[Byo])
            P.actv(junk, yo[:], AF.Square, reads=[Byo], writes=[Btmpo, Bfst], accum_out=fst[:, i, 0:1])
            P.ts("dve", fst[:, i, 1:2], fst[:, i, 0:1], 1.0 / D, EPS, ALU.mult, ALU.add, reads=[Bfst], writes=[Bfst])
            P.actv(fst[:, i, 1:2], fst[:, i, 1:2], AF.Ln, reads=[Bfst], writes=[Bfst])
            P.actv(fst[:, i, 2:3], fst[:, i, 1:2], AF.Exp, reads=[Bfst], writes=[Bfst], scale=-0.5)
            P.stt("dve", yo[:], yo[:], fst[:, i, 2:3], fgb[:], ALU.mult, ALU.mult, reads=[Byo, Bfst, Bk1], writes=[Byo])
            o = P.dma(dst_d[i * 128:(i + 1) * 128, :], yo[:], reads=[Byo], q="sp")
            final_ops.append(o)
            ck("N7")
        A.top = top0
        A.release(mL)

    try:
        if mode == "l0":
            layer_gdn(x_in, out_d)
        elif mode == "l1":
            layer_nsa(x_in, out_d)
        else:
            layer_gdn(x_in, x1_d)
            layer_nsa(x1_d, out_d)
    except _Stop:
        P.barrier()
        final_ops.append(P.dma(out_d[0:128, 0:16], cst[:, 0, 0:16], reads=[Bcst]))
    P.emit(final_dma_ops=final_ops + tap_ops)
    if os.environ.get("KVERBOSE"):
        print("arena peak bytes", getattr(A, "peak", 0), "of", nc.sbuf_top - A.base, "est_us", getattr(P, "est_time", 0.0))
    es.close()
    return nc


def _rel_bucket(d):
    n = np.maximum(d, 0)
    nf = np.maximum(n, 1).astype(np.float32)
    large = 16 + (np.log(nf / np.float32(16)) / np.float32(math.log(128 / 16)) * np.float32(16)).astype(np.int32)
    large = np.minimum(large, 31)
    return np.where(n < 16, n, large)


def make_nsa_consts():
    c = {}
    d = np.arange(384) - 128
    bk = _rel_bucket(d)
    oh = np.zeros((33, 384), np.float32)
    for j in range(384):
        if d[j] >= 0:
            oh[bk[j], j] += 1.0
            oh[31, j] -= 1.0
        else:
            oh[32, j] = -BIG
    c["nsa_oh"] = oh
    sh = np.zeros((128, NT, 127), np.float32)
    for i in range(NT):
        for blk in range(127):
            rp = blk - 8 * i
            if -8 <= rp <= 6:
                sh[rp + 8, i, blk] = 1.0
            elif rp >= 7:
                sh[15, i, blk] = 1.0
    c["nsa_sh"] = sh
    em = np.zeros((128, NT, 128), np.float32)
    for j in range(NT):
        for key in range(128):
            em[2 * j + key // 64, j, key] = 1.0
    c["nsa_em"] = em
    p = np.arange(128)
    c["nsa_wm"] = np.where(p[:, None] > p[None, :], 0.0, -BIG).astype(np.float32)
    cm = np.zeros((128, NT, 32), np.float32)
    fb = np.zeros((128, NT, 32), np.float32)
    for i in range(NT):
        q = 128 * i + p
        cur = q // 64
        s = np.arange(32)
        forced = (s[None, :] == 0) | (s[None, :] == cur[:, None]) | (s[None, :] == cur[:, None] - 1)
        causal = s[None, :] * 64 <= q[:, None]
        cm[:, i, :] = (causal & ~forced) * 1.0
        fb[:, i, :] = np.where(forced, 1e30, np.where(causal, 0.0, -1e30))
    c["nsa_cmask"] = cm
    c["nsa_fbias"] = fb
    cs = np.arange(127)[:, None] * 16
    ss = np.arange(32)[None, :] * 64
    ov = np.clip(np.minimum(cs + 32, ss + 64) - np.maximum(cs, ss), 0, None).astype(np.float32) / 32.0
    c["nsa_ovl"] = np.ascontiguousarray(ov)
    return c


def _permute_nsa_w(w):
    cols = []
    for g in range(4):
        for hd in (4 * g, 4 * g + 2, 4 * g + 1, 4 * g + 3):
            cols += list(range(hd * 64, hd * 64 + 64))
    kv0 = 1024

    def kvcols(i, g):
        return list(range(kv0 + i * 256 + g * 64, kv0 + i * 256 + g * 64 + 64))
    for i in (2, 4):
        for g in range(4):
            cols += kvcols(i, g) + kvcols(i, g)
    for i in (0, 1):
        for g in range(4):
            cols += kvcols(i, g)
    for i in (3, 5):
        for g in range(4):
            cols += kvcols(i, g)
    off = 1024 + 6 * 256
    cols += list(range(off + 48, off + 48 + 1024))
    for br in range(3):
        for hd in range(16):
            cols.append(off + hd * 3 + br)
    return np.ascontiguousarray(w[:, np.asarray(cols)])


_CACHE = {}


def _prep_common(inputs):
    f = lambda a: np.ascontiguousarray(np.asarray(a, dtype=np.float32))
    com = {
        "ada_w": f(inputs["ada_w"]),
        "ada_b": f(np.asarray(inputs["ada_b"]).reshape(2, 24, 128).transpose(0, 2, 1)),
        "ada_b_row": f(inputs["ada_b"]),
        "norm_g": f(np.asarray(inputs["norm_g"]).reshape(2, 8, 128).transpose(0, 2, 1)),
        "gdn_w_in": f(inputs["gdn_w_in"][0]),
        "gdn_cw": f(np.asarray(inputs["gdn_conv_w"][0]).reshape(4, 32, 128).transpose(2, 1, 0)),
        "gdn_alog": f(np.asarray(inputs["gdn_a_log"]).reshape(1, 16)),
        "gdn_dtb": f(np.asarray(inputs["gdn_dt_bias"]).reshape(1, 16)),
        "gdn_normw": f(np.asarray(inputs["gdn_norm_w"]).reshape(1, 128)),
        "gdn_w_out": f(inputs["gdn_w_out"][0]),
        "final_g": f(np.asarray(inputs["final_g"]).reshape(1, D)),
        "cst": make_consts(),
        "nsa_w": _permute_nsa_w(f(inputs["nsa_w_in"][0])),
        "nsa_w1": f(inputs["nsa_cmp_w1"][0]),
        "nsa_w2": f(inputs["nsa_cmp_w2"][0]),
        "nsa_pos": f(inputs["nsa_cmp_pos"][0]),
        "nsa_wo": f(inputs["nsa_w_out"][0]),
        "rel_bias": f(inputs["rel_bias"]),
    }
    com.update(make_nsa_consts())
    return com


def kernel(**inputs):
    x = np.asarray(inputs["x"], dtype=np.float32)
    c = np.asarray(inputs["c"], dtype=np.float32)
    com = _prep_common(inputs)
    if "all" not in _CACHE:
        _CACHE["all"] = build("all")
    nc = _CACHE["all"]
    in_maps = []
    for b in range(8):
        m = dict(com)
        m["x"] = np.ascontiguousarray(x[b])
        m["cvec"] = np.ascontiguousarray(c[b].reshape(8, 128).T)
        in_maps.append(m)
    res = run_bass_kernel_spmd(nc, in_maps, core_ids=list(range(8)))
    return np.stack([res.results[b]["out"] for b in range(8)], axis=0)
```
